# Optimizing a Trainium2 kernel written in Bass

```python
import math
import jax, jax.numpy as jnp
from jax import lax
import numpy as np

D_MODEL = 4096
BATCH = 2
SEQ = 8192
DEPTH = 2

N_META = 16
POOL_WINDOWS = (2, 4, 8, 16)
N_POOL_GROUPS = len(POOL_WINDOWS)
POOL_GROUP = D_MODEL // N_POOL_GROUPS
HEAD_DIM = 128
N_HEADS = D_MODEL // (2 * HEAD_DIM)
V_HEAD_DIM = 2 * HEAD_DIM
D_QK = N_HEADS * 2 * HEAD_DIM
D_V = N_HEADS * V_HEAD_DIM
Q_BLOCK = 128
D_FF = 14336 * D_MODEL // 4096
N_EXPERTS = 8
TOP_K = 2
D_FF_EXPERT = D_FF // 4
RMS_EPS = 1e-6
SUBLN_EPS = 1e-5
MASK_VALUE = -1e30

kernel_name = "hybrid_pool_diffattn_moe_trunk"


def rms_norm(x, g, eps=RMS_EPS):
    xf = x.astype(jnp.float32)
    y = xf * lax.rsqrt(jnp.mean(xf * xf, axis=-1, keepdims=True) + eps)
    return (y * g.astype(jnp.float32)).astype(x.dtype)


def causal_window_mean(h, w):
    L = h.shape[1]
    cs = jnp.cumsum(h.astype(jnp.float32), axis=1)
    cs_shift = jnp.pad(cs, ((0, 0), (w, 0), (0, 0)))[:, :L]
    cnt = jnp.minimum(jnp.arange(L) + 1, w).astype(jnp.float32)
    return ((cs - cs_shift) / cnt[None, :, None]).astype(h.dtype)


def pool_mixer(u, pool_w, pool_scale):
    B, L, _ = u.shape
    g = u.reshape(B, L, N_POOL_GROUPS, POOL_GROUP)
    pooled = jnp.stack([causal_window_mean(g[:, :, i], w) for i, w in enumerate(POOL_WINDOWS)], axis=2) - g
    out = jnp.einsum('blgc,gcd->blgd', pooled, pool_w).reshape(B, L, D_MODEL)
    return out * pool_scale


def diff_lambda_init(layer_idx):
    return 0.8 - 0.6 * math.exp(-0.3 * layer_idx)


def diff_attention(u, w_qkv, q_norm, k_norm, lq1, lk1, lq2, lk2, subln, w_o, lambda_init):
    B, L, _ = u.shape
    qkv = u @ w_qkv
    q = qkv[..., :D_QK].reshape(B, L, N_HEADS, 2, HEAD_DIM)
    k = qkv[..., D_QK:2 * D_QK].reshape(B, L, N_HEADS, 2, HEAD_DIM)
    v = qkv[..., 2 * D_QK:].reshape(B, L, N_HEADS, V_HEAD_DIM)
    q = rms_norm(q, q_norm)
    k = rms_norm(k, k_norm)
    lam = (jnp.exp(jnp.sum(lq1.astype(jnp.float32) * lk1.astype(jnp.float32)))
           - jnp.exp(jnp.sum(lq2.astype(jnp.float32) * lk2.astype(jnp.float32)))
           + lambda_init)
    pad_front = (-N_META) % Q_BLOCK
    q = jnp.pad(q, ((0, 0), (pad_front, 0), (0, 0), (0, 0), (0, 0)))
    k = jnp.pad(k, ((0, 0), (pad_front, 0), (0, 0), (0, 0), (0, 0)))
    v = jnp.pad(v, ((0, 0), (pad_front, 0), (0, 0), (0, 0)))
    Lp = L + pad_front
    n_blocks = Lp // Q_BLOCK
    kpos = jnp.arange(Lp)
    scale = HEAD_DIM ** -0.5

    def one_block(qb):
        start = qb * Q_BLOCK
        qblk = lax.dynamic_slice_in_dim(q, start, Q_BLOCK, axis=1)
        s = jnp.einsum('bqhmd,bkhmd->bhmqk', qblk, k, preferred_element_type=jnp.float32) * scale
        qpos = start + jnp.arange(Q_BLOCK)
        mask = (kpos[None, :] <= qpos[:, None]) & (kpos[None, :] >= pad_front)
        p = jax.nn.softmax(jnp.where(mask, s, MASK_VALUE), axis=-1)
        a = p[:, :, 0] - lam * p[:, :, 1]
        return jnp.einsum('bhqk,bkhe->bqhe', a.astype(v.dtype), v)

    o = lax.map(one_block, jnp.arange(n_blocks))
    o = jnp.moveaxis(o, 0, 1).reshape(B, Lp, N_HEADS, V_HEAD_DIM)[:, pad_front:]
    o = rms_norm(o, subln, SUBLN_EPS) * (1.0 - lambda_init)
    return o.reshape(B, L, D_V) @ w_o


def swiglu(u, w_gate, w_up, w_down):
    return (jax.nn.silu(u @ w_gate) * (u @ w_up)) @ w_down


def moe_swiglu(u, router, e_gate, e_up, e_down):
    B, L, D = u.shape
    t = u.reshape(B * L, D)
    logits = (t @ router).astype(jnp.float32)
    top_v, top_i = lax.top_k(logits, TOP_K)
    gates = jax.nn.softmax(top_v, axis=-1)
    comb = jnp.sum(jax.nn.one_hot(top_i, N_EXPERTS, dtype=jnp.float32) * gates[..., None], axis=1)
    out = jnp.zeros((B * L, D), jnp.float32)
    for e in range(N_EXPERTS):
        out = out + comb[:, e:e + 1] * swiglu(t, e_gate[e], e_up[e], e_down[e]).astype(jnp.float32)
    return out.astype(u.dtype).reshape(B, L, D)


def setup_inputs(seed: int = 0) -> dict:
    key = jax.random.key(seed)
    ks = jax.random.split(key, 24)
    n_even = (DEPTH + 1) // 2
    n_odd = DEPTH // 2
    f32 = jnp.float32

    def nrm(k, shape, s):
        return jax.random.normal(k, shape, f32) * s

    return {
        "x": nrm(ks[0], (BATCH, SEQ, D_MODEL), 1.0),
        "meta_tokens": nrm(ks[1], (N_META, D_MODEL), 1.0),
        "norm_mix": 1.0 + nrm(ks[2], (DEPTH, D_MODEL), 0.02),
        "norm_ffn": 1.0 + nrm(ks[3], (DEPTH, D_MODEL), 0.02),
        "pool_w": nrm(ks[4], (n_even, N_POOL_GROUPS, POOL_GROUP, POOL_GROUP), POOL_GROUP ** -0.5),
        "pool_scale": 1.0 + nrm(ks[5], (n_even, D_MODEL), 0.02),
        "ffn_w_gate": nrm(ks[6], (n_even, D_MODEL, D_FF), D_MODEL ** -0.5),
        "ffn_w_up": nrm(ks[7], (n_even, D_MODEL, D_FF), D_MODEL ** -0.5),
        "ffn_w_down": nrm(ks[8], (n_even, D_FF, D_MODEL), D_FF ** -0.5),
        "w_qkv": nrm(ks[9], (n_odd, D_MODEL, 2 * D_QK + D_V), D_MODEL ** -0.5),
        "q_norm": 1.0 + nrm(ks[10], (n_odd, HEAD_DIM), 0.02),
        "k_norm": 1.0 + nrm(ks[11], (n_odd, HEAD_DIM), 0.02),
        "lambda_q1": nrm(ks[12], (n_odd, HEAD_DIM), 0.1),
        "lambda_k1": nrm(ks[13], (n_odd, HEAD_DIM), 0.1),
        "lambda_q2": nrm(ks[14], (n_odd, HEAD_DIM), 0.1),
        "lambda_k2": nrm(ks[15], (n_odd, HEAD_DIM), 0.1),
        "subln": 1.0 + nrm(ks[16], (n_odd, V_HEAD_DIM), 0.02),
        "w_o": nrm(ks[17], (n_odd, D_V, D_MODEL), D_V ** -0.5),
        "router": nrm(ks[18], (n_odd, D_MODEL, N_EXPERTS), D_MODEL ** -0.5),
        "exp_w_gate": nrm(ks[19], (n_odd, N_EXPERTS, D_MODEL, D_FF_EXPERT), D_MODEL ** -0.5),
        "exp_w_up": nrm(ks[20], (n_odd, N_EXPERTS, D_MODEL, D_FF_EXPERT), D_MODEL ** -0.5),
        "exp_w_down": nrm(ks[21], (n_odd, N_EXPERTS, D_FF_EXPERT, D_MODEL), D_FF_EXPERT ** -0.5),
    }


def reference(x, meta_tokens, norm_mix, norm_ffn, pool_w, pool_scale, ffn_w_gate, ffn_w_up, ffn_w_down,
              w_qkv, q_norm, k_norm, lambda_q1, lambda_k1, lambda_q2, lambda_k2, subln, w_o,
              router, exp_w_gate, exp_w_up, exp_w_down):
    B = x.shape[0]
    meta = jnp.broadcast_to(meta_tokens.astype(x.dtype)[None], (B, N_META, D_MODEL))
    h = jnp.concatenate([meta, x], axis=1)
    for i in range(DEPTH):
        j = i // 2
        u = rms_norm(h, norm_mix[i])
        if i % 2 == 0:
            h = h + pool_mixer(u, pool_w[j], pool_scale[j])
        else:
            h = h + diff_attention(u, w_qkv[j], q_norm[j], k_norm[j], lambda_q1[j], lambda_k1[j],
                                   lambda_q2[j], lambda_k2[j], subln[j], w_o[j], diff_lambda_init(i))
        u = rms_norm(h, norm_ffn[i])
        if i % 2 == 0:
            h = h + swiglu(u, ffn_w_gate[j], ffn_w_up[j], ffn_w_down[j])
        else:
            h = h + moe_swiglu(u, router[j], exp_w_gate[j], exp_w_up[j], exp_w_down[j])
    return h[:, N_META:]
```

```python
import math
import numpy as np
import concourse.bass as bass
import concourse.mybir as mybir
from concourse.bass_utils import run_bass_kernel_spmd

F32 = mybir.dt.float32
BF16 = mybir.dt.bfloat16
AF = mybir.ActivationFunctionType
ALU = mybir.AluOpType

N_CORES = 8


class Prog:
    def __init__(self, nc, same_engine_sync=True):
        self.nc = nc
        self.ins = []
        self.last_writer = {}
        self.readers = {}
        self.same_engine_sync = same_engine_sync

    def add(self, eng, fn, reads=(), writes=(), dma=None):
        idx = len(self.ins)
        deps = set()
        for b in reads:
            w = self.last_writer.get(b)
            if w is not None:
                deps.add(w)
        for b in writes:
            w = self.last_writer.get(b)
            if w is not None:
                deps.add(w)
            deps.update(self.readers.get(b, ()))
        deps.discard(idx)
        fdeps = []
        for d in deps:
            p = self.ins[d]
            if p["dma"] is None and p["eng"] == eng:
                if eng == "pe" or not self.same_engine_sync:
                    continue
            fdeps.append(d)
        self.ins.append(dict(eng=eng, fn=fn, deps=sorted(fdeps), dma=dma))
        for b in reads:
            self.readers.setdefault(b, []).append(idx)
        for b in writes:
            self.last_writer[b] = idx
            self.readers[b] = []
        return idx

    def emit(self, final_wait_keys=()):
        nc = self.nc
        needed = set()
        for it in self.ins:
            needed.update(it["deps"])
        semkeys = []
        for it in self.ins:
            k = ("dma", it["dma"]) if it["dma"] is not None else ("eng", it["eng"])
            if k not in semkeys:
                semkeys.append(k)
        engs = ["pe", "act", "dve", "pool", "sp"]
        eng_obj = dict(pe=nc.tensor, act=nc.scalar, dve=nc.vector, pool=nc.gpsimd, sp=nc.sync)
        from contextlib import ExitStack
        with ExitStack() as st:
            sems = {}
            for i, k in enumerate(semkeys):
                sems[k] = st.enter_context(nc.semaphore("s%d" % i))
            cnt = {k: 0 for k in semkeys}
            ev = {}
            for idx, it in enumerate(self.ins):
                if it["dma"] is not None:
                    k = ("dma", it["dma"])
                    cnt[k] += 16
                    ev[idx] = (k, cnt[k], 16)
                elif idx in needed:
                    k = ("eng", it["eng"])
                    cnt[k] += 1
                    ev[idx] = (k, cnt[k], 1)
            streams = {e: [] for e in engs}
            waited = {e: {} for e in engs}
            for idx, it in enumerate(self.ins):
                e = it["eng"]
                waits = []
                for d in it["deps"]:
                    k, v, _ = ev[d]
                    if waited[e].get(k, 0) >= v:
                        continue
                    waited[e][k] = v
                    waits.append((k, v))
                streams[e].append((waits, it["fn"], ev.get(idx)))
            finals = [(("dma", k), cnt[("dma", k)]) for k in final_wait_keys if ("dma", k) in cnt]
            block = st.enter_context(nc.Block())

            def run(e):
                eo = eng_obj[e]
                for waits, fn, evv in streams[e]:
                    for k, v in waits:
                        eo.wait_ge(sems[k], v)
                    ins = fn(eo)
                    if evv is not None:
                        ins.then_inc(sems[evv[0]], evv[2])
                if e == "sp":
                    for k, v in finals:
                        eo.wait_ge(sems[k], v)

            @block.tensor
            def _(x):
                run("pe")

            @block.scalar
            def _(x):
                run("act")

            @block.vector
            def _(x):
                run("dve")

            @block.gpsimd
            def _(x):
                run("pool")

            @block.sync
            def _(x):
                run("sp")


class Ctx:
    def __init__(self, nc, st, prog):
        self.nc, self.st, self.p = nc, st, prog
        self.ps = [st.enter_context(nc.psum_tensor("ps%d" % i, [128, 512], F32)) for i in range(8)]
        self.ps_i = 0
        self.n = 0

    def sb(self, shape, dt, name=None):
        self.n += 1
        return self.st.enter_context(self.nc.sbuf_tensor(name or ("t%d" % self.n), shape, dt))

    def bank(self):
        rot = getattr(self, "rot", None) or list(range(8))
        i = rot[self.ps_i % len(rot)]
        self.ps_i += 1
        return i


def emit_consts(cx, inv_d_list):
    p = cx.p
    out = {}
    for v in inv_d_list:
        t = cx.sb([128, 128], F32)
        p.add("pool", lambda e, t=t, v=v: e.memset(t[:], float(v)), writes=[("c", id(t))])
        out[v] = (t, ("c", id(t)))
    return out


def get_eps(cx, eps):
    d = cx.__dict__.setdefault("_eps", {})
    if eps not in d:
        t = cx.sb([128, 1], F32)
        cx.p.add("pool", lambda e: e.memset(t[:], float(eps)), writes=[("eps", eps)])
        d[eps] = (t, ("eps", eps))
    return d[eps]


def emit_rmsnorm(cx, h, hkey, KC, n0, N, g_t, gkey, ones, oneskey, eps, out, okey, ocol0, sq, sqkeys, rstd, rstdkey,
                 h_c0=0, extra_scale=None):
    p = cx.p
    b = cx.bank()
    ps = cx.ps[b]
    for c in range(KC):
        s = sq[c % 2]
        sk = sqkeys[c % 2]
        p.add("act", lambda e, s=s, c=c: e.activation(out=s[:, 0:N], in_=h[:, h_c0 + c, n0:n0 + N], func=AF.Square),
              reads=[hkey], writes=[sk])
        p.add("pe", lambda e, s=s, c=c: e.matmul(ps[:, 0:N], ones[:, :], s[:, 0:N], start=(c == 0), stop=(c == KC - 1)),
              reads=[sk, oneskey], writes=[("ps", b)])
    epst, epskey = get_eps(cx, eps)
    p.add("act", lambda e: e.activation(out=rstd[:, 0:N], in_=ps[:, 0:N], func=AF.Sqrt, bias=epst[:, 0:1], scale=1.0),
          reads=[("ps", b), epskey], writes=[rstdkey])
    p.add("dve", lambda e: e.reciprocal(out=rstd[:, 0:N], in_=rstd[:, 0:N]), reads=[rstdkey], writes=[rstdkey])
    for c in range(KC):
        if extra_scale is None:
            p.add("dve", lambda e, c=c: e.scalar_tensor_tensor(out=out[:, c, ocol0:ocol0 + N], in0=h[:, h_c0 + c, n0:n0 + N],
                                                               scalar=g_t[:, c:c + 1], in1=rstd[:, 0:N],
                                                               op0=ALU.mult, op1=ALU.mult),
                  reads=[hkey, rstdkey, gkey], writes=[okey])
        else:
            raise NotImplementedError


def dma_w(cx, eng, dst, dkey, src, semkey, reads=()):
    cx.p.add(eng, lambda e: e.dma_start(out=dst, in_=src, max_dma_last_dim=8192), reads=list(reads), writes=[dkey],
             dma=semkey)


class Cfg1:
    def __init__(self, D=4096, F=14336, NT=512, NTILES=4, FG=14, windows=(2, 4, 8, 16), eps=1e-6):
        self.D, self.F, self.NT, self.NTILES, self.FG = D, F, NT, NTILES, FG
        self.KC = D // 128
        self.FT = F // 128
        self.NG = self.FT // FG
        assert self.FT % FG == 0
        self.windows = windows
        self.GC = self.KC // len(windows)
        self.T = 16 + NT * NTILES
        self.eps = eps


def build_l1(cfg):
    nc = bass.Bass("TRN2", target_bir_lowering=False)
    D, F, NT, KC, FT, FG, NG, GC, T = cfg.D, cfg.F, cfg.NT, cfg.KC, cfg.FT, cfg.FG, cfg.NG, cfg.GC, cfg.T
    NW = len(cfg.windows)
    xT = nc.dram_tensor("xT", [128, KC, T], F32, kind="ExternalInput").ap()
    gmix = nc.dram_tensor("gmix", [128, KC], F32, kind="ExternalInput").ap()
    gffn = nc.dram_tensor("gffn", [128, KC], F32, kind="ExternalInput").ap()
    pscale = nc.dram_tensor("pscale", [128, KC], F32, kind="ExternalInput").ap()
    poolw = nc.dram_tensor("poolw", [NW, GC, 128, GC, 128], F32, kind="ExternalInput").ap()
    wg = nc.dram_tensor("wg", [FT, 128, KC, 128], F32, kind="ExternalInput").ap()
    wu = nc.dram_tensor("wu", [FT, 128, KC, 128], F32, kind="ExternalInput").ap()
    wd = nc.dram_tensor("wd", [NG, KC, 128, FG, 128], F32, kind="ExternalInput").ap()
    hT = nc.dram_tensor("hT", [128, KC, T], F32, kind="ExternalOutput").ap()

    from contextlib import ExitStack
    with ExitStack() as st:
        p = Prog(nc)
        cx = Ctx(nc, st, p)
        h = cx.sb([128, KC, NT], F32, "h")
        ub = cx.sb([128, KC, 16 + NT], BF16, "ub")
        carry = cx.sb([128, KC, 16], BF16, "carry")
        xb = cx.sb([128, max(FG, GC), NT], BF16, "xb")
        tmpA = cx.sb([128, 16 + NT], F32, "tmpA")
        tmpB = cx.sb([128, 16 + NT], F32, "tmpB")
        WS = 4
        wslot = [cx.sb([128, max(KC, FG), 128], BF16, "w%d" % i) for i in range(WS)]
        sq = [cx.sb([128, NT], F32, "sq%d" % i) for i in range(2)]
        rstd = cx.sb([128, NT], F32, "rstd")
        sg = [cx.sb([128, NT], F32, "sg%d" % i) for i in range(2)]
        g1 = cx.sb([128, KC], F32, "g1")
        g2 = cx.sb([128, KC], F32, "g2")
        psc = cx.sb([128, KC], F32, "psc")
        rc = cx.sb([128, NW, 16], F32, "rc")
        consts = emit_consts(cx, [1.0 / D])
        ones, oneskey = consts[1.0 / D]

        p.add("sp", lambda e: e.dma_start(out=g1[:], in_=gmix), writes=["g1"], dma="c1")
        p.add("sp", lambda e: e.dma_start(out=g2[:], in_=gffn), writes=["g2"], dma="c2")
        p.add("sp", lambda e: e.dma_start(out=psc[:], in_=pscale), writes=["psc"], dma="c3")
        for wi, w in enumerate(cfg.windows):
            p.add("pool", lambda e, wi=wi, w=w: e.memset(rc[:, wi, :], 1.0 / w), writes=["rc"])
            for t in range(min(w - 1, 16)):
                p.add("pool", lambda e, wi=wi, t=t: e.memset(rc[:, wi, t:t + 1], 1.0 / (t + 1)), writes=["rc"])
        p.add("pool", lambda e: e.memset(carry[:], 0.0), writes=["carry"])

        wi_ctr = [0]

        def next_w():
            i = wi_ctr[0] % WS
            wi_ctr[0] += 1
            return wslot[i], ("w", i), "w%d" % i

        tiles = [(0, 16)] + [(16 + i * NT, NT) for i in range(cfg.NTILES)]
        for ti, (n0, N) in enumerate(tiles):
            first = (ti == 0)
            p.add("sp", lambda e, n0=n0, N=N: e.dma_start(out=h[:, :, 0:N], in_=xT[:, :, n0:n0 + N]),
                  writes=["h"], dma="h")
            p.add("pool", lambda e: e.tensor_copy(out=ub[:, :, 0:16], in_=carry[:]), reads=["carry"], writes=["ub"])
            emit_rmsnorm(cx, h, "h", KC, 0, N, g1, "g1", ones, oneskey, cfg.eps, ub, "ub", 16,
                         sq, ["sq0", "sq1"], rstd, "rstd")
            p.add("pool", lambda e, N=N: e.tensor_copy(out=carry[:], in_=ub[:, :, N:N + 16]), reads=["ub"], writes=["carry"])
            for gi, w in enumerate(cfg.windows):
                nsteps = int(math.log2(w))
                for cl in range(GC):
                    c = gi * GC + cl
                    src = None
                    lo = 0
                    bufs = [tmpA, tmpB]
                    keys = ["tmpA", "tmpB"]
                    for s_ in range(nsteps):
                        sh = 1 << s_
                        dst = bufs[s_ % 2]
                        dk = keys[s_ % 2]
                        nlo = lo + sh
                        if src is None:
                            p.add("pool", lambda e, dst=dst, c=c, nlo=nlo, sh=sh, N=N: e.tensor_tensor(
                                out=dst[:, nlo:16 + N], in0=ub[:, c, nlo:16 + N], in1=ub[:, c, nlo - sh:16 + N - sh], op=ALU.add),
                                reads=["ub"], writes=[dk])
                        else:
                            sk = keys[(s_ - 1) % 2]
                            p.add("pool", lambda e, dst=dst, src=src, nlo=nlo, sh=sh, N=N: e.tensor_tensor(
                                out=dst[:, nlo:16 + N], in0=src[:, nlo:16 + N], in1=src[:, nlo - sh:16 + N - sh], op=ALU.add),
                                reads=[sk], writes=[dk])
                        src = dst
                        lo = nlo
                    sk = keys[(nsteps - 1) % 2]
                    if first:
                        ok = keys[nsteps % 2]
                        o2 = bufs[nsteps % 2]
                        p.add("pool", lambda e, src=src, o2=o2, gi=gi, N=N: e.tensor_tensor(
                            out=o2[:, 16:16 + N], in0=src[:, 16:16 + N], in1=rc[:, gi, 0:N], op=ALU.mult),
                            reads=[sk, "rc"], writes=[ok])
                        p.add("pool", lambda e, o2=o2, cl=cl, c=c, N=N: e.tensor_tensor(
                            out=xb[:, cl, 0:N], in0=o2[:, 16:16 + N], in1=ub[:, c, 16:16 + N], op=ALU.subtract),
                            reads=[ok, "ub"], writes=["xb"])
                    else:
                        p.add("dve", lambda e, src=src, cl=cl, c=c, w=w, N=N: e.scalar_tensor_tensor(
                            out=xb[:, cl, 0:N], in0=src[:, 16:16 + N], scalar=1.0 / w, in1=ub[:, c, 16:16 + N],
                            op0=ALU.mult, op1=ALU.subtract), reads=[sk, "ub"], writes=["xb"])
                for dt in range(GC):
                    wt, wk, wsem = next_w()
                    dma_w(cx, "pool", wt[:, 0:GC, :], wk, poolw[gi, dt], wsem)
                    b = cx.bank()
                    ps = cx.ps[b]
                    for kc in range(GC):
                        p.add("pe", lambda e, ps=ps, wt=wt, kc=kc, N=N: e.matmul(ps[:, 0:N], wt[:, kc, :], xb[:, kc, 0:N],
                                                                                 start=(kc == 0), stop=(kc == GC - 1)),
                              reads=[wk, "xb"], writes=[("ps", b)])
                    c = gi * GC + dt
                    p.add("dve", lambda e, ps=ps, c=c, N=N: e.scalar_tensor_tensor(
                        out=h[:, c, 0:N], in0=ps[:, 0:N], scalar=psc[:, c:c + 1], in1=h[:, c, 0:N], op0=ALU.mult, op1=ALU.add),
                        reads=[("ps", b), "psc", "h"], writes=["h"])
            emit_rmsnorm(cx, h, "h", KC, 0, N, g2, "g2", ones, oneskey, cfg.eps, ub, "ub", 0,
                         sq, ["sq0", "sq1"], rstd, "rstd")
            emit_ffn(cx, cfg, ub, "ub", 0, N, xb, "xb", h, "h", wg, wu, wd, None, next_w, sg, FT, FG, KC)
            p.add("sp", lambda e, n0=n0, N=N: e.dma_start(out=hT[:, :, n0:n0 + N], in_=h[:, :, 0:N]),
                  reads=["h"], dma="out")
        p.emit(final_wait_keys=["out"])
    return nc


def emit_ffn(cx, cfg, u, ukey, ucol0, N, xb, xbkey, h, hkey, wg, wu, wd, colscale, next_w, sg, FT, FG, KC):
    p = cx.p
    NG = FT // FG
    for grp in range(NG):
        for fl in range(FG):
            ft = grp * FG + fl
            wgt, wgk, wgs = next_w()
            dma_w(cx, "pool", wgt[:, 0:KC, :], wgk, wg[ft], wgs)
            wut, wuk, wus = next_w()
            dma_w(cx, "pool", wut[:, 0:KC, :], wuk, wu[ft], wus)
            bg = cx.bank()
            bu = cx.bank()
            psg, psu = cx.ps[bg], cx.ps[bu]
            for kc in range(KC):
                p.add("pe", lambda e, psg=psg, wgt=wgt, kc=kc: e.matmul(psg[:, 0:N], wgt[:, kc, :], u[:, kc, ucol0:ucol0 + N],
                                                                        start=(kc == 0), stop=(kc == KC - 1)),
                      reads=[wgk, ukey], writes=[("ps", bg)])
            for kc in range(KC):
                p.add("pe", lambda e, psu=psu, wut=wut, kc=kc: e.matmul(psu[:, 0:N], wut[:, kc, :], u[:, kc, ucol0:ucol0 + N],
                                                                        start=(kc == 0), stop=(kc == KC - 1)),
                      reads=[wuk, ukey], writes=[("ps", bu)])
            s = sg[ft % 2]
            sk = "sg%d" % (ft % 2)
            p.add("act", lambda e, s=s, psg=psg: e.activation(out=s[:, 0:N], in_=psg[:, 0:N], func=AF.Silu),
                  reads=[("ps", bg)], writes=[sk])
            p.add("dve", lambda e, s=s, psu=psu, fl=fl: e.tensor_tensor(out=xb[:, fl, 0:N], in0=psu[:, 0:N], in1=s[:, 0:N],
                                                                        op=ALU.mult),
                  reads=[("ps", bu), sk], writes=[xbkey])
            if colscale is not None:
                cs, csk = colscale
                p.add("pool", lambda e, fl=fl, cs=cs: e.tensor_tensor(out=xb[:, fl, 0:N], in0=xb[:, fl, 0:N], in1=cs,
                                                                      op=ALU.mult),
                      reads=[xbkey, csk], writes=[xbkey])
        for dt in range(KC):
            wt, wk, wsem = next_w()
            dma_w(cx, "pool", wt[:, 0:FG, :], wk, wd[grp, dt], wsem)
            b = cx.bank()
            ps = cx.ps[b]
            for fl in range(FG):
                p.add("pe", lambda e, ps=ps, wt=wt, fl=fl: e.matmul(ps[:, 0:N], wt[:, fl, :], xb[:, fl, 0:N],
                                                                    start=(fl == 0), stop=(fl == FG - 1)),
                      reads=[wk, xbkey], writes=[("ps", b)])
            p.add("dve", lambda e, ps=ps, dt=dt: e.tensor_tensor(out=h[:, dt, 0:N], in0=ps[:, 0:N], in1=h[:, dt, 0:N], op=ALU.add),
                  reads=[("ps", b), hkey], writes=[hkey])


def vec_t(v):
    return np.ascontiguousarray(np.asarray(v, np.float32).reshape(-1, 128).T)


def w_tiles(W):
    K, Fd = W.shape
    return np.ascontiguousarray(np.asarray(W, np.float32).reshape(K // 128, 128, Fd // 128, 128).transpose(2, 1, 0, 3))


def wd_tiles(W, FG):
    Fd, D = W.shape
    NG = Fd // 128 // FG
    return np.ascontiguousarray(np.asarray(W, np.float32).reshape(NG, FG, 128, D // 128, 128).transpose(0, 3, 2, 1, 4))


def to_fm(x):
    T, D = x.shape
    return np.ascontiguousarray(np.asarray(x, np.float32).reshape(T, D // 128, 128).transpose(2, 1, 0))


def from_fm(xT):
    P, KC, T = xT.shape
    return np.ascontiguousarray(xT.transpose(2, 1, 0).reshape(T, KC * 128))


def run_l1(cfg, seqs, meta, norm_mix0, norm_ffn0, pool_w0, pool_scale0, wg0, wu0, wd0):
    nc = build_l1(cfg)
    GC = cfg.GC
    NW = len(cfg.windows)
    pw = np.asarray(pool_w0, np.float32)
    pwt = np.ascontiguousarray(pw.reshape(NW, GC, 128, GC, 128).transpose(0, 3, 2, 1, 4))
    shared = dict(gmix=vec_t(norm_mix0), gffn=vec_t(norm_ffn0), pscale=vec_t(pool_scale0), poolw=pwt,
                  wg=w_tiles(wg0), wu=w_tiles(wu0), wd=wd_tiles(wd0, cfg.FG))
    in_maps = [dict(shared, xT=to_fm(s)) for s in seqs]
    res = run_bass_kernel_spmd(nc, in_maps, core_ids=list(range(len(seqs))))
    return [from_fm(r["hT"]) for r in res.results]


class Cfg2:
    def __init__(self, D=4096, HPC=4, LQ=8192, NT=512, eps=1e-6, sub_eps=1e-5, lambda_init=0.0):
        self.D, self.HPC, self.LQ, self.NT = D, HPC, LQ, NT
        self.KC = D // 128
        self.L = 16 + LQ
        self.NB = LQ // 128
        self.NCH = LQ // NT
        self.eps, self.sub_eps, self.lambda_init = eps, sub_eps, lambda_init


def build_l2(cfg):
    nc = bass.Bass("TRN2", target_bir_lowering=False)
    D, HPC, LQ, NT, KC, L, NB, NCH = cfg.D, cfg.HPC, cfg.LQ, cfg.NT, cfg.KC, cfg.L, cfg.NB, cfg.NCH
    BPC = NT // 128
    hT = nc.dram_tensor("hT", [128, KC, L], F32, kind="ExternalInput").ap()
    gmix = nc.dram_tensor("gmix", [128, KC], F32, kind="ExternalInput").ap()
    wqkv = nc.dram_tensor("wqkv", [HPC, 6, 128, KC, 128], F32, kind="ExternalInput").ap()
    qkn = nc.dram_tensor("qkn", [128, 2], F32, kind="ExternalInput").ap()
    lvec = nc.dram_tensor("lvec", [128, 4], F32, kind="ExternalInput").ap()
    subl = nc.dram_tensor("subl", [128, 2], F32, kind="ExternalInput").ap()
    tri_d = nc.dram_tensor("tri", [128, 128], F32, kind="ExternalInput").ap()
    oT = nc.dram_tensor("oT", [HPC, 2, 128, LQ], F32, kind="ExternalOutput").ap()
    uT = nc.dram_tensor("uT_scratch", [128, KC, L], BF16).ap()

    from contextlib import ExitStack
    with ExitStack() as st:
        p = Prog(nc)
        cx = Ctx(nc, st, p)
        NA = 128
        h = cx.sb([128, KC, NA], F32, "h")
        ub = cx.sb([128, KC, NT], BF16, "ub")
        sq = [cx.sb([128, NT], F32, "sq%d" % i) for i in range(2)]
        rstd = cx.sb([128, NT], F32, "rstd")
        g1 = cx.sb([128, KC], F32, "g1")
        qk = cx.sb([128, 2], F32, "qk")
        lv = cx.sb([128, 4], F32, "lv")
        sl = cx.sb([128, 2], F32, "sl")
        tri = cx.sb([128, 128], BF16, "tri_sb")
        onesb = cx.sb([128, 128], BF16, "onesb")
        pr = cx.sb([128, 2], F32, "pr")
        ex = cx.sb([128, 2], F32, "ex")
        neglam = cx.sb([128, 1], F32, "neglam")
        consts = emit_consts(cx, [1.0 / D, 1.0 / 128, 1.0 / 256, 1.0])
        onesD, onesDk = consts[1.0 / D]
        ones128, ones128k = consts[1.0 / 128]
        ones256, ones256k = consts[1.0 / 256]
        ones1, ones1k = consts[1.0]
        p.add("sp", lambda e: e.dma_start(out=g1[:], in_=gmix), writes=["g1"], dma="c1")
        p.add("sp", lambda e: e.dma_start(out=qk[:], in_=qkn), writes=["qk"], dma="c2")
        p.add("sp", lambda e: e.dma_start(out=lv[:], in_=lvec), writes=["lv"], dma="c3")
        p.add("sp", lambda e: e.dma_start(out=sl[:], in_=subl), writes=["sl"], dma="c4")
        p.add("pool", lambda e: e.dma_start(out=tri[:], in_=tri_d), writes=["tri"], dma="c5")
        p.add("pool", lambda e: e.memset(onesb[:], 1.0), writes=["onesb"])
        p.add("dve", lambda e: e.tensor_scalar(out=qk[:, 0:1], in0=qk[:, 0:1], scalar1=float(128 ** -0.5), scalar2=None, op0=ALU.mult),
              reads=["qk"], writes=["qk"])
        p.add("dve", lambda e: e.tensor_scalar(out=sl[:], in0=sl[:], scalar1=float(1.0 - cfg.lambda_init), scalar2=None, op0=ALU.mult),
              reads=["sl"], writes=["sl"])
        p.add("dve", lambda e: e.tensor_tensor(out=pr[:, 0:1], in0=lv[:, 0:1], in1=lv[:, 1:2], op=ALU.mult), reads=["lv"], writes=["pr"])
        p.add("dve", lambda e: e.tensor_tensor(out=pr[:, 1:2], in0=lv[:, 2:3], in1=lv[:, 3:4], op=ALU.mult), reads=["lv"], writes=["pr"])
        b = cx.bank()
        psl = cx.ps[b]
        p.add("pe", lambda e: e.matmul(psl[:, 0:2], ones1[:, :], pr[:, 0:2], start=True, stop=True), reads=["pr", ones1k], writes=[("ps", b)])
        p.add("act", lambda e: e.activation(out=ex[:], in_=psl[:, 0:2], func=AF.Exp), reads=[("ps", b)], writes=["ex"])
        p.add("dve", lambda e: e.tensor_tensor(out=neglam[:], in0=ex[:, 1:2], in1=ex[:, 0:1], op=ALU.subtract), reads=["ex"], writes=["neglam"])
        p.add("dve", lambda e: e.tensor_scalar(out=neglam[:], in0=neglam[:], scalar1=float(-cfg.lambda_init), scalar2=None, op0=ALU.add),
              reads=["neglam"], writes=["neglam"])

        tiles = [(0, 16)] + [(16 + i * NT, NT) for i in range(NCH)]
        tilesA = [(0, 16)] + [(16 + i * NA, NA) for i in range(LQ // NA)]
        for ti, (n0, N) in enumerate(tilesA):
            p.add("sp", lambda e, n0=n0, N=N: e.dma_start(out=h[:, :, 0:N], in_=hT[:, :, n0:n0 + N]), writes=["h"], dma="h")
            emit_rmsnorm(cx, h, "h", KC, 0, N, g1, "g1", onesD, onesDk, cfg.eps, ub, "ub", 0, sq, ["sq0", "sq1"], rstd, "rstd")
            p.add("sp", lambda e, n0=n0, N=N: e.dma_start(out=uT[:, :, n0:n0 + N], in_=ub[:, :, 0:N]), reads=["ub"],
                  writes=["uT"], dma="us")

        KT = cx.sb([128, 2, L], BF16, "KT")
        V = cx.sb([128, NB + 1, 256], BF16, "V")
        wsl = [cx.sb([128, KC, 128], BF16, "wq%d" % i) for i in range(4)]
        qc = cx.sb([128, 2, NT], BF16, "qc")
        et = [cx.sb([128, NT], BF16, "et%d" % i) for i in range(3)]
        rden = cx.sb([128, NT], F32, "rden")
        o1n = cx.sb([128, 2, NT], F32, "o1n")
        otmp = cx.sb([128, 2, NT], F32, "otmp")
        ofin, oout = otmp, o1n
        cx.rot = [3, 4, 5, 6, 7]
        cx.ps_i = 0
        ACC = (0, 1, 2)

        def qknorm(ps, psb, N, gcol, out_ap, okey):
            s = sq[0]
            p.add("act", lambda e: e.activation(out=s[:, 0:N], in_=ps[:, 0:N], func=AF.Square), reads=[("ps", psb)], writes=["sq0"])
            b2 = cx.bank()
            ps2 = cx.ps[b2]
            p.add("pe", lambda e: e.matmul(ps2[:, 0:N], ones128[:, :], s[:, 0:N], start=True, stop=True), reads=["sq0", ones128k],
                  writes=[("ps", b2)])
            epst, epskey = get_eps(cx, cfg.eps)
            p.add("act", lambda e: e.activation(out=rstd[:, 0:N], in_=ps2[:, 0:N], func=AF.Sqrt, bias=epst[:, 0:1], scale=1.0),
                  reads=[("ps", b2), epskey], writes=["rstd"])
            p.add("dve", lambda e: e.reciprocal(out=rstd[:, 0:N], in_=rstd[:, 0:N]), reads=["rstd"], writes=["rstd"])
            p.add("dve", lambda e: e.scalar_tensor_tensor(out=out_ap, in0=ps[:, 0:N], scalar=qk[:, gcol:gcol + 1], in1=rstd[:, 0:N],
                                                          op0=ALU.mult, op1=ALU.mult),
                  reads=[("ps", psb), "rstd", "qk"], writes=[okey])

        for hh in range(HPC):
            for i, wi in enumerate((2, 3, 4, 5)):
                dma_w(cx, "pool", wsl[i][:], ("wq", i), wqkv[hh, wi], "wq%d" % i)
            for ti, (n0, N) in enumerate(tiles):
                p.add("sp", lambda e, n0=n0, N=N: e.dma_start(out=ub[:, :, 0:N], in_=uT[:, :, n0:n0 + N]), reads=["uT"],
                      writes=["ub"], dma="ul")
                for m in range(2):
                    b = cx.bank()
                    ps = cx.ps[b]
                    for kc in range(KC):
                        p.add("pe", lambda e, ps=ps, m=m, kc=kc, N=N: e.matmul(ps[:, 0:N], wsl[m][:, kc, :], ub[:, kc, 0:N],
                                                                               start=(kc == 0), stop=(kc == KC - 1)),
                              reads=[("wq", m), "ub"], writes=[("ps", b)])
                    qknorm(ps, b, N, 1, KT[:, m, n0:n0 + N], "KT")
                nblk = max(1, N // 128)
                for bi in range(nblk):
                    M = min(128, N)
                    blk = 0 if ti == 0 else 1 + (n0 - 16) // 128 + bi
                    b = cx.bank()
                    ps = cx.ps[b]
                    for half in range(2):
                        for kc in range(KC):
                            p.add("pe", lambda e, ps=ps, kc=kc, half=half, bi=bi, M=M: e.matmul(
                                ps[0:M, half * 128:(half + 1) * 128], ub[:, kc, bi * 128:bi * 128 + M], wsl[2 + half][:, kc, :],
                                start=(kc == 0), stop=(kc == KC - 1)),
                                reads=[("wq", 2 + half), "ub"], writes=[("ps", b)])
                    p.add("act", lambda e, ps=ps, blk=blk, M=M: e.copy(out=V[0:M, blk, :], in_=ps[0:M, 0:256]), reads=[("ps", b)],
                          writes=["V"])
            for i, wi in enumerate((0, 1)):
                dma_w(cx, "pool", wsl[i][:], ("wq", i), wqkv[hh, wi], "wq%d" % i)
            for c in range(NCH):
                q0 = 16 + c * NT
                p.add("sp", lambda e, q0=q0: e.dma_start(out=ub[:, :, 0:NT], in_=uT[:, :, q0:q0 + NT]), reads=["uT"],
                      writes=["ub"], dma="ul")
                for m in range(2):
                    b = cx.bank()
                    ps = cx.ps[b]
                    for kc in range(KC):
                        p.add("pe", lambda e, ps=ps, m=m, kc=kc: e.matmul(ps[:, 0:NT], wsl[m][:, kc, :], ub[:, kc, 0:NT],
                                                                          start=(kc == 0), stop=(kc == KC - 1)),
                              reads=[("wq", m), "ub"], writes=[("ps", b)])
                    qknorm(ps, b, NT, 0, qc[:, m, :], "qc")
                for m in range(2):
                    kts = [(0, 16, 0, 0, False)]
                    for kb in range(BPC * c):
                        kts.append((16 + kb * 128, 128, 1 + kb, 0, False))
                    for i in range(BPC):
                        kb = BPC * c + i
                        kts.append((16 + kb * 128, 128, 1 + kb, 128 * i, True))
                    for ki, (k0, M, blk, qlo, masked) in enumerate(kts):
                        bs = cx.bank()
                        pss = cx.ps[bs]
                        e_t = et[ki % 3]
                        ek = "et%d" % (ki % 3)
                        p.add("pe", lambda e, pss=pss, m=m, k0=k0, M=M, qlo=qlo: e.matmul(
                            pss[0:M, qlo:NT], KT[:, m, k0:k0 + M], qc[:, m, qlo:NT], start=True, stop=True),
                            reads=["KT", "qc"], writes=[("ps", bs)])
                        p.add("act", lambda e, pss=pss, e_t=e_t, M=M, qlo=qlo: e.activation(out=e_t[0:M, qlo:NT], in_=pss[0:M, qlo:NT],
                                                                                            func=AF.Exp),
                              reads=[("ps", bs)], writes=[ek])
                        if masked:
                            p.add("pool", lambda e, e_t=e_t, qlo=qlo: e.tensor_tensor(out=e_t[:, qlo:qlo + 128], in0=e_t[:, qlo:qlo + 128],
                                                                                      in1=tri[:, :], op=ALU.mult),
                                  reads=[ek, "tri"], writes=[ek])
                        first, last = (ki == 0), (ki == len(kts) - 1)
                        for ec in range(2):
                            p.add("pe", lambda e, ec=ec, e_t=e_t, M=M, blk=blk, qlo=qlo, first=first, last=last: e.matmul(
                                cx.ps[ACC[ec]][:, qlo:NT], V[0:M, blk, ec * 128:(ec + 1) * 128], e_t[0:M, qlo:NT], start=first, stop=last),
                                reads=["V", ek], writes=[("ps", ACC[ec])])
                        p.add("pe", lambda e, e_t=e_t, M=M, qlo=qlo, first=first, last=last: e.matmul(
                            cx.ps[ACC[2]][:, qlo:NT], onesb[0:M, :], e_t[0:M, qlo:NT], start=first, stop=last),
                            reads=["onesb", ek], writes=[("ps", ACC[2])])
                    p.add("dve", lambda e: e.reciprocal(out=rden[:, :], in_=cx.ps[ACC[2]][:, 0:NT]), reads=[("ps", ACC[2])], writes=["rden"])
                    dstt, dk = (o1n, "o1n") if m == 0 else (otmp, "otmp")
                    for ec in range(2):
                        p.add("dve", lambda e, ec=ec, dstt=dstt: e.tensor_tensor(out=dstt[:, ec, :], in0=cx.ps[ACC[ec]][:, 0:NT], in1=rden[:, :],
                                                                                 op=ALU.mult),
                              reads=[("ps", ACC[ec]), "rden"], writes=[dk])
                for ec in range(2):
                    p.add("dve", lambda e, ec=ec: e.scalar_tensor_tensor(out=ofin[:, ec, :], in0=otmp[:, ec, :], scalar=neglam[:, 0:1],
                                                                         in1=o1n[:, ec, :], op0=ALU.mult, op1=ALU.add),
                          reads=["otmp", "o1n", "neglam"], writes=["otmp"])
                bsn = cx.bank()
                psn = cx.ps[bsn]
                for ec in range(2):
                    s = sq[ec]
                    p.add("act", lambda e, s=s, ec=ec: e.activation(out=s[:, 0:NT], in_=ofin[:, ec, :], func=AF.Square), reads=["otmp"],
                          writes=["sq%d" % ec])
                    p.add("pe", lambda e, s=s, ec=ec: e.matmul(psn[:, 0:NT], ones256[:, :], s[:, 0:NT], start=(ec == 0), stop=(ec == 1)),
                          reads=["sq%d" % ec, ones256k], writes=[("ps", bsn)])
                epst, epskey = get_eps(cx, cfg.sub_eps)
                p.add("act", lambda e: e.activation(out=rstd[:, 0:NT], in_=psn[:, 0:NT], func=AF.Sqrt, bias=epst[:, 0:1], scale=1.0),
                      reads=[("ps", bsn), epskey], writes=["rstd"])
                p.add("dve", lambda e: e.reciprocal(out=rstd[:, 0:NT], in_=rstd[:, 0:NT]), reads=["rstd"], writes=["rstd"])
                for ec in range(2):
                    p.add("dve", lambda e, ec=ec: e.scalar_tensor_tensor(out=oout[:, ec, :], in0=ofin[:, ec, :], scalar=sl[:, ec:ec + 1],
                                                                         in1=rstd[:, 0:NT], op0=ALU.mult, op1=ALU.mult),
                          reads=["otmp", "rstd", "sl"], writes=["o1n"])
                    p.add("sp", lambda e, ec=ec, hh=hh, c=c: e.dma_start(out=oT[hh, ec, :, c * NT:(c + 1) * NT], in_=oout[:, ec, :]),
                          reads=["o1n"], dma="out")
        p.emit(final_wait_keys=["out"])
    return nc


def qkv_tiles(w_qkv, heads, n_heads):
    W = np.asarray(w_qkv, np.float32)
    D = W.shape[0]
    KC = D // 128
    DQK = n_heads * 256
    out = np.empty((len(heads), 6, 128, KC, 128), np.float32)
    for i, hd in enumerate(heads):
        cols = [hd * 256, hd * 256 + 128, DQK + hd * 256, DQK + hd * 256 + 128, 2 * DQK + hd * 256, 2 * DQK + hd * 256 + 128]
        for j, c0 in enumerate(cols):
            out[i, j] = W[:, c0:c0 + 128].reshape(KC, 128, 128).transpose(1, 0, 2)
    return out


def run_l2(cfg, h1_list, norm_mix1, w_qkv, q_norm, k_norm, lq1, lk1, lq2, lk2, subln, n_heads, head_groups=None):
    nc = build_l2(cfg)
    HPC = cfg.HPC
    ngrp = n_heads // HPC
    tri = np.triu(np.ones((128, 128), np.float32))
    shared = dict(gmix=vec_t(norm_mix1),
                  qkn=np.ascontiguousarray(np.stack([q_norm, k_norm], axis=1).astype(np.float32)),
                  lvec=np.ascontiguousarray(np.stack([lq1, lk1, lq2, lk2], axis=1).astype(np.float32)),
                  subl=np.ascontiguousarray(np.asarray(subln, np.float32).reshape(2, 128).T), tri=tri)
    in_maps = []
    hfm = [to_fm(hb) for hb in h1_list]
    wts = [qkv_tiles(w_qkv, list(range(g * HPC, (g + 1) * HPC)), n_heads) for g in range(ngrp)]
    for b in range(len(h1_list)):
        for g in range(ngrp):
            in_maps.append(dict(shared, hT=hfm[b], wqkv=wts[g]))
    res = run_bass_kernel_spmd(nc, in_maps, core_ids=list(range(len(in_maps))))
    outs = []
    i = 0
    for b in range(len(h1_list)):
        parts = []
        for g in range(ngrp):
            o = res.results[i]["oT"]
            i += 1
            parts.append(o.transpose(3, 0, 1, 2).reshape(cfg.LQ, HPC * 256))
        outs.append(np.concatenate(parts, axis=1))
    return outs


class Cfg3:
    def __init__(self, D=4096, FE=3584, NE=8, NT=512, NTILES=4, FG=7, eps=1e-6):
        self.D, self.FE, self.NE, self.NT, self.NTILES, self.FG = D, FE, NE, NT, NTILES, FG
        self.KC = D // 128
        self.FT = FE // 128
        assert self.FT % FG == 0
        self.NG = self.FT // FG
        self.T = NT * NTILES
        self.eps = eps


def build_l3(cfg):
    nc = bass.Bass("TRN2", target_bir_lowering=False)
    D, FE, NE, NT, KC, FT, FG, NG, T = cfg.D, cfg.FE, cfg.NE, cfg.NT, cfg.KC, cfg.FT, cfg.FG, cfg.NG, cfg.T
    hT = nc.dram_tensor("hT", [128, KC, T], F32, kind="ExternalInput").ap()
    oT = nc.dram_tensor("oT", [128, KC, T], F32, kind="ExternalInput").ap()
    gffn = nc.dram_tensor("gffn", [128, KC], F32, kind="ExternalInput").ap()
    wo = nc.dram_tensor("wo", [KC, 128, KC, 128], F32, kind="ExternalInput").ap()
    rt = nc.dram_tensor("rt", [128, KC, NE], F32, kind="ExternalInput").ap()
    ident_d = nc.dram_tensor("ident", [128, 128], F32, kind="ExternalInput").ap()
    weg = nc.dram_tensor("weg", [NE, FT, 128, KC, 128], F32, kind="ExternalInput").ap()
    weu = nc.dram_tensor("weu", [NE, FT, 128, KC, 128], F32, kind="ExternalInput").ap()
    wed = nc.dram_tensor("wed", [NE, NG, KC, 128, FG, 128], F32, kind="ExternalInput").ap()
    outT = nc.dram_tensor("outT", [128, KC, T], F32, kind="ExternalOutput").ap()
    dbg = nc.dram_tensor("dbg", [128, NE, T], F32, kind="ExternalOutput").ap() if getattr(cfg, "debug", False) else None

    from contextlib import ExitStack
    with ExitStack() as st:
        p = Prog(nc)
        cx = Ctx(nc, st, p)
        h = cx.sb([128, KC, NT], F32, "h")
        ub = cx.sb([128, KC, NT], BF16, "ub")
        xb = cx.sb([128, FG, NT], BF16, "xb")
        WS = 4
        wslot = [cx.sb([128, max(KC, FG), 128], BF16, "w%d" % i) for i in range(WS)]
        sq = [cx.sb([128, NT], F32, "sq%d" % i) for i in range(2)]
        rstd = cx.sb([128, NT], F32, "rstd")
        sg = [cx.sb([128, NT], F32, "sg%d" % i) for i in range(2)]
        g2 = cx.sb([128, KC], F32, "g2")
        rtb = cx.sb([128, KC, NE], F32, "rtb")
        uf = [cx.sb([128, 128], F32, "uf%d" % i) for i in range(2)]
        ident = cx.sb([128, 128], F32, "ident_sb")
        combb = cx.sb([128, NE, NT], F32, "combb")
        lg = cx.sb([128, NE], F32, "lg")
        lg2 = cx.sb([128, NE], F32, "lg2")
        eq1 = cx.sb([128, NE], F32, "eq1")
        eq2 = cx.sb([128, NE], F32, "eq2")
        comb_all = cx.sb([128, NT // 128, NE], F32, "comb_all")
        sm = cx.sb([128, 8], F32, "sm")
        diag = [cx.sb([128, 128], F32, "diag%d" % i) for i in range(2)]
        consts = emit_consts(cx, [1.0 / D, 1.0])
        onesD, onesDk = consts[1.0 / D]
        ones1, ones1k = consts[1.0]
        p.add("sp", lambda e: e.dma_start(out=g2[:], in_=gffn), writes=["g2"], dma="c1")
        p.add("sp", lambda e: e.dma_start(out=ident[:], in_=ident_d), writes=["ident"], dma="c2")
        p.add("sp", lambda e: e.dma_start(out=rtb[:], in_=rt), writes=["rtb"], dma="c3")
        wi_ctr = [0]

        def next_w():
            i = wi_ctr[0] % WS
            wi_ctr[0] += 1
            return wslot[i], ("w", i), "w%d" % i

        AX = mybir.AxisListType.X
        for ti in range(cfg.NTILES):
            n0 = ti * NT
            N = NT
            p.add("sp", lambda e, n0=n0: e.dma_start(out=h[:, :, :], in_=hT[:, :, n0:n0 + NT]), writes=["h"], dma="h")
            for half in range(2):
                c0, c1 = half * KC // 2, (half + 1) * KC // 2
                p.add("pool", lambda e, n0=n0, c0=c0, c1=c1: e.dma_start(out=ub[:, c0:c1, :], in_=oT[:, c0:c1, n0:n0 + NT],
                                                                         max_dma_last_dim=8192),
                      writes=["ub"], dma="o")
            for dt in range(KC):
                wt, wk, wsem = next_w()
                dma_w(cx, "pool", wt[:, 0:KC, :], wk, wo[dt], wsem)
                b = cx.bank()
                ps = cx.ps[b]
                for kc in range(KC):
                    p.add("pe", lambda e, ps=ps, wt=wt, kc=kc: e.matmul(ps[:, 0:N], wt[:, kc, :], ub[:, kc, 0:N],
                                                                        start=(kc == 0), stop=(kc == KC - 1)),
                          reads=[wk, "ub"], writes=[("ps", b)])
                p.add("dve", lambda e, ps=ps, dt=dt: e.tensor_tensor(out=h[:, dt, 0:N], in0=ps[:, 0:N], in1=h[:, dt, 0:N], op=ALU.add),
                      reads=[("ps", b), "h"], writes=["h"])
            emit_rmsnorm(cx, h, "h", KC, 0, N, g2, "g2", onesD, onesDk, cfg.eps, ub, "ub", 0, sq, ["sq0", "sq1"], rstd, "rstd")
            cx.rot = [0, 1, 2, 3]
            cb = [4, 5, 6, 7]
            for tb in range(NT // 128):
                b = cx.bank()
                ps = cx.ps[b]
                for kc in range(KC):
                    uft = uf[kc % 2]
                    ufk = "uf%d" % (kc % 2)
                    p.add("dve", lambda e, uft=uft, kc=kc, tb=tb: e.scalar_tensor_tensor(
                        out=uft[:, :], in0=h[:, kc, tb * 128:(tb + 1) * 128], scalar=g2[:, kc:kc + 1],
                        in1=rstd[:, tb * 128:(tb + 1) * 128], op0=ALU.mult, op1=ALU.mult),
                        reads=["h", "g2", "rstd"], writes=[ufk])
                    p.add("pe", lambda e, ps=ps, kc=kc, uft=uft: e.matmul(ps[:, 0:NE], uft[:, :], rtb[:, kc, :],
                                                                          start=(kc == 0), stop=(kc == KC - 1)),
                          reads=[ufk, "rtb"], writes=[("ps", b)])
                p.add("dve", lambda e, ps=ps: e.tensor_copy(out=lg[:], in_=ps[:, 0:NE]), reads=[("ps", b)], writes=["lg"])
                p.add("dve", lambda e: e.tensor_reduce(out=sm[:, 0:1], in_=lg[:], axis=AX, op=ALU.max), reads=["lg"], writes=["sm"])
                p.add("dve", lambda e: e.tensor_scalar(out=eq1[:], in0=lg[:], scalar1=sm[:, 0:1], scalar2=None, op0=ALU.is_equal),
                      reads=["lg", "sm"], writes=["eq1"])
                p.add("dve", lambda e: e.scalar_tensor_tensor(out=lg2[:], in0=eq1[:], scalar=-1e30, in1=lg[:], op0=ALU.mult, op1=ALU.add),
                      reads=["eq1", "lg"], writes=["lg2"])
                p.add("dve", lambda e: e.tensor_reduce(out=sm[:, 1:2], in_=lg2[:], axis=AX, op=ALU.max), reads=["lg2"], writes=["sm"])
                p.add("dve", lambda e: e.tensor_scalar(out=eq2[:], in0=lg2[:], scalar1=sm[:, 1:2], scalar2=None, op0=ALU.is_equal),
                      reads=["lg2", "sm"], writes=["eq2"])
                p.add("dve", lambda e: e.tensor_tensor(out=sm[:, 2:3], in0=sm[:, 1:2], in1=sm[:, 0:1], op=ALU.subtract), reads=["sm"], writes=["sm"])
                p.add("act", lambda e: e.activation(out=sm[:, 3:4], in_=sm[:, 2:3], func=AF.Exp), reads=["sm"], writes=["sm"])
                p.add("dve", lambda e: e.tensor_scalar(out=sm[:, 4:5], in0=sm[:, 3:4], scalar1=1.0, scalar2=None, op0=ALU.add), reads=["sm"], writes=["sm"])
                p.add("dve", lambda e: e.reciprocal(out=sm[:, 5:6], in_=sm[:, 4:5]), reads=["sm"], writes=["sm"])
                p.add("dve", lambda e: e.tensor_tensor(out=sm[:, 6:7], in0=sm[:, 3:4], in1=sm[:, 5:6], op=ALU.mult), reads=["sm"], writes=["sm"])
                p.add("dve", lambda e, tb=tb: e.tensor_scalar(out=comb_all[:, tb, :], in0=eq1[:], scalar1=sm[:, 5:6], scalar2=None, op0=ALU.mult),
                      reads=["eq1", "sm"], writes=["comb"])
                p.add("dve", lambda e, tb=tb: e.scalar_tensor_tensor(out=comb_all[:, tb, :], in0=eq2[:], scalar=sm[:, 6:7], in1=comb_all[:, tb, :], op0=ALU.mult, op1=ALU.add),
                      reads=["eq2", "sm", "comb"], writes=["comb"])
            for eh in range(0, NE, 4):
                for ex_ in range(eh, min(NE, eh + 4)):
                    for tb in range(NT // 128):
                        dg = diag[(ex_ * (NT // 128) + tb) % 2]
                        dk = "diag%d" % ((ex_ * (NT // 128) + tb) % 2)
                        p.add("dve", lambda e, dg=dg, ex_=ex_, tb=tb: e.tensor_scalar(out=dg[:], in0=ident[:], scalar1=comb_all[:, tb, ex_:ex_ + 1],
                                                                                      scalar2=None, op0=ALU.mult),
                              reads=["ident", "comb"], writes=[dk])
                        p.add("pe", lambda e, dg=dg, ex_=ex_, tb=tb: e.matmul(cx.ps[cb[ex_ % 4]][:, tb * 128:(tb + 1) * 128], ones1[:, :], dg[:, :],
                                                                              start=True, stop=True),
                              reads=[dk, ones1k], writes=[("ps", cb[ex_ % 4])])
                    p.add("act", lambda e, ex_=ex_: e.copy(out=combb[:, ex_, :], in_=cx.ps[cb[ex_ % 4]][:, 0:NT]), reads=[("ps", cb[ex_ % 4])],
                          writes=[("combb", ex_)])
            cx.rot = None
            if dbg is not None:
                p.add("sp", lambda e, n0=n0: e.dma_start(out=dbg[:, :, n0:n0 + NT], in_=combb[:, :, :]),
                      reads=[("combb", i) for i in range(NE)], dma="out")
            for ex_ in range(NE):
                emit_ffn(cx, cfg, ub, "ub", 0, N, xb, "xb", h, "h", weg[ex_], weu[ex_], wed[ex_], (combb[:, ex_, :], ("combb", ex_)),
                         next_w, sg, FT, FG, KC)
            p.add("sp", lambda e, n0=n0: e.dma_start(out=outT[:, :, n0:n0 + NT], in_=h[:, :, :]), reads=["h"], dma="out")
        p.emit(final_wait_keys=["out"])
    return nc


def run_l3(cfg, h_list, o_list, norm_ffn1, w_o, router, eg, eu, ed):
    nc = build_l3(cfg)
    NE = cfg.NE
    KC = cfg.KC
    shared = dict(gffn=vec_t(norm_ffn1), wo=w_tiles(w_o),
                  rt=np.ascontiguousarray(np.asarray(router, np.float32).reshape(KC, 128, NE).transpose(1, 0, 2)),
                  ident=np.eye(128, dtype=np.float32),
                  weg=np.stack([w_tiles(eg[e]) for e in range(NE)]),
                  weu=np.stack([w_tiles(eu[e]) for e in range(NE)]),
                  wed=np.stack([wd_tiles(ed[e], cfg.FG) for e in range(NE)]))
    in_maps = [dict(shared, hT=to_fm(hh), oT=to_fm(oo)) for hh, oo in zip(h_list, o_list)]
    res = run_bass_kernel_spmd(nc, in_maps, core_ids=list(range(len(in_maps))))
    if getattr(cfg, "debug", False):
        return [from_fm(r["outT"]) for r in res.results], [r["dbg"] for r in res.results]
    return [from_fm(r["outT"]) for r in res.results]


def kernel(x, meta_tokens, norm_mix, norm_ffn, pool_w, pool_scale, ffn_w_gate, ffn_w_up, ffn_w_down,
           w_qkv, q_norm, k_norm, lambda_q1, lambda_k1, lambda_q2, lambda_k2, subln, w_o,
           router, exp_w_gate, exp_w_up, exp_w_down):
    x = np.asarray(x, np.float32)
    meta = np.asarray(meta_tokens, np.float32)
    B, S, D = x.shape
    NG = N_CORES // B
    per = S // NG
    full = [np.concatenate([meta, x[b]], axis=0) for b in range(B)]
    cfg1 = Cfg1(D=D, F=np.asarray(ffn_w_gate).shape[-1], NT=512, NTILES=per // 512, FG=14)
    seqs = [full[b][per * j: per * j + 16 + per] for b in range(B) for j in range(NG)]
    p1 = run_l1(cfg1, seqs, meta, np.asarray(norm_mix)[0], np.asarray(norm_ffn)[0], np.asarray(pool_w)[0],
                np.asarray(pool_scale)[0], np.asarray(ffn_w_gate)[0], np.asarray(ffn_w_up)[0], np.asarray(ffn_w_down)[0])
    h1 = [np.concatenate([p1[NG * b][0:16]] + [p1[NG * b + j][16:] for j in range(NG)], axis=0) for b in range(B)]
    del p1, seqs, full
    n_heads = 16
    cfg2 = Cfg2(D=D, HPC=n_heads // NG, LQ=S, NT=512, lambda_init=0.8 - 0.6 * math.exp(-0.3 * 1))
    o = run_l2(cfg2, h1, np.asarray(norm_mix)[1], np.asarray(w_qkv)[0], np.asarray(q_norm)[0], np.asarray(k_norm)[0],
               np.asarray(lambda_q1)[0], np.asarray(lambda_k1)[0], np.asarray(lambda_q2)[0], np.asarray(lambda_k2)[0],
               np.asarray(subln)[0], n_heads=n_heads)
    cfg3 = Cfg3(D=D, FE=np.asarray(exp_w_gate).shape[-1], NE=np.asarray(exp_w_gate).shape[1], NT=512, NTILES=per // 512, FG=7)
    h_list = [h1[b][16 + per * j: 16 + per * (j + 1)] for b in range(B) for j in range(NG)]
    o_list = [o[b][per * j: per * (j + 1)] for b in range(B) for j in range(NG)]
    outs = run_l3(cfg3, h_list, o_list, np.asarray(norm_ffn)[1], np.asarray(w_o)[0], np.asarray(router)[0],
                  np.asarray(exp_w_gate)[0], np.asarray(exp_w_up)[0], np.asarray(exp_w_down)[0])
    out = np.stack([np.concatenate(outs[NG * b: NG * (b + 1)], axis=0) for b in range(B)])
    return np.ascontiguousarray(out.astype(np.float32))
```

```python
import math
import numpy as np
import concourse.bass as bass
import concourse.mybir as mybir
from concourse.bass_utils import run_bass_kernel_spmd

F32 = mybir.dt.float32
BF16 = mybir.dt.bfloat16
AF = mybir.ActivationFunctionType
ALU = mybir.AluOpType

N_CORES = 8


class Prog:
    def __init__(self, nc, same_engine_sync=True):
        self.nc = nc
        self.ins = []
        self.last_writer = {}
        self.readers = {}
        self.same_engine_sync = same_engine_sync

    def add(self, eng, fn, reads=(), writes=(), dma=None):
        idx = len(self.ins)
        deps = set()
        for b in reads:
            w = self.last_writer.get(b)
            if w is not None:
                deps.add(w)
        for b in writes:
            w = self.last_writer.get(b)
            if w is not None:
                deps.add(w)
            deps.update(self.readers.get(b, ()))
        deps.discard(idx)
        fdeps = []
        for d in deps:
            p = self.ins[d]
            if p["dma"] is None and p["eng"] == eng:
                if eng == "pe" or not self.same_engine_sync:
                    continue
            fdeps.append(d)
        self.ins.append(dict(eng=eng, fn=fn, deps=sorted(fdeps), dma=dma))
        for b in reads:
            self.readers.setdefault(b, []).append(idx)
        for b in writes:
            self.last_writer[b] = idx
            self.readers[b] = []
        return idx

    def emit(self, final_wait_keys=()):
        nc = self.nc
        needed = set()
        for it in self.ins:
            needed.update(it["deps"])
        semkeys = []
        for it in self.ins:
            k = ("dma", it["dma"]) if it["dma"] is not None else ("eng", it["eng"])
            if k not in semkeys:
                semkeys.append(k)
        engs = ["pe", "act", "dve", "pool", "sp"]
        eng_obj = dict(pe=nc.tensor, act=nc.scalar, dve=nc.vector, pool=nc.gpsimd, sp=nc.sync)
        from contextlib import ExitStack
        with ExitStack() as st:
            sems = {}
            for i, k in enumerate(semkeys):
                sems[k] = st.enter_context(nc.semaphore("s%d" % i))
            cnt = {k: 0 for k in semkeys}
            ev = {}
            for idx, it in enumerate(self.ins):
                if it["dma"] is not None:
                    k = ("dma", it["dma"])
                    cnt[k] += 16
                    ev[idx] = (k, cnt[k], 16)
                elif idx in needed:
                    k = ("eng", it["eng"])
                    cnt[k] += 1
                    ev[idx] = (k, cnt[k], 1)
            streams = {e: [] for e in engs}
            waited = {e: {} for e in engs}
            for idx, it in enumerate(self.ins):
                e = it["eng"]
                waits = []
                for d in it["deps"]:
                    k, v, _ = ev[d]
                    if waited[e].get(k, 0) >= v:
                        continue
                    waited[e][k] = v
                    waits.append((k, v))
                streams[e].append((waits, it["fn"], ev.get(idx)))
            finals = [(("dma", k), cnt[("dma", k)]) for k in final_wait_keys if ("dma", k) in cnt]
            block = st.enter_context(nc.Block())

            def run(e):
                eo = eng_obj[e]
                for waits, fn, evv in streams[e]:
                    for k, v in waits:
                        eo.wait_ge(sems[k], v)
                    ins = fn(eo)
                    if evv is not None:
                        ins.then_inc(sems[evv[0]], evv[2])
                if e == "sp":
                    for k, v in finals:
                        eo.wait_ge(sems[k], v)

            @block.tensor
            def _(x):
                run("pe")

            @block.scalar
            def _(x):
                run("act")

            @block.vector
            def _(x):
                run("dve")

            @block.gpsimd
            def _(x):
                run("pool")

            @block.sync
            def _(x):
                run("sp")


class Ctx:
    def __init__(self, nc, st, prog):
        self.nc, self.st, self.p = nc, st, prog
        self.ps = [st.enter_context(nc.psum_tensor("ps%d" % i, [128, 512], F32)) for i in range(8)]
        self.ps_i = 0
        self.n = 0

    def sb(self, shape, dt, name=None):
        self.n += 1
        return self.st.enter_context(self.nc.sbuf_tensor(name or ("t%d" % self.n), shape, dt))

    def bank(self):
        rot = getattr(self, "rot", None) or list(range(8))
        i = rot[self.ps_i % len(rot)]
        self.ps_i += 1
        return i


def emit_consts(cx, inv_d_list):
    p = cx.p
    out = {}
    for v in inv_d_list:
        t = cx.sb([128, 128], F32)
        p.add("pool", lambda e, t=t, v=v: e.memset(t[:], float(v)), writes=[("c", id(t))])
        out[v] = (t, ("c", id(t)))
    return out


def get_eps(cx, eps):
    d = cx.__dict__.setdefault("_eps", {})
    if eps not in d:
        t = cx.sb([128, 1], F32)
        cx.p.add("pool", lambda e: e.memset(t[:], float(eps)), writes=[("eps", eps)])
        d[eps] = (t, ("eps", eps))
    return d[eps]


def emit_rmsnorm(cx, h, hkey, KC, n0, N, g_t, gkey, ones, oneskey, eps, out, okey, ocol0, sq, sqkeys, rstd, rstdkey,
                 h_c0=0, extra_scale=None):
    p = cx.p
    b = cx.bank()
    ps = cx.ps[b]
    G = max(1, min(KC, int(sq[0].shape[-1]) // N))
    while KC % G:
        G -= 1
    for gi, c0 in enumerate(range(0, KC, G)):
        s = sq[gi % 2]
        sk = sqkeys[gi % 2]
        if G == 1:
            p.add("act", lambda e, s=s, c=c0: e.activation(out=s[:, 0:N], in_=h[:, h_c0 + c, n0:n0 + N], func=AF.Square),
                  reads=[hkey], writes=[sk])
        else:
            p.add("act", lambda e, s=s, c=c0: e.activation(out=s[:, 0:G * N].rearrange("p (g n) -> p g n", g=G),
                                                           in_=h[:, h_c0 + c:h_c0 + c + G, n0:n0 + N], func=AF.Square),
                  reads=[hkey], writes=[sk])
        for j in range(G):
            c = c0 + j
            p.add("pe", lambda e, s=s, c=c, j=j: e.matmul(ps[:, 0:N], ones[:, :], s[:, j * N:(j + 1) * N], start=(c == 0), stop=(c == KC - 1)),
                  reads=[sk, oneskey], writes=[("ps", b)])
    epst, epskey = get_eps(cx, eps)
    p.add("act", lambda e: e.activation(out=rstd[:, 0:N], in_=ps[:, 0:N], func=AF.Sqrt, bias=epst[:, 0:1], scale=1.0),
          reads=[("ps", b), epskey], writes=[rstdkey])
    p.add("dve", lambda e: e.reciprocal(out=rstd[:, 0:N], in_=rstd[:, 0:N]), reads=[rstdkey], writes=[rstdkey])
    for c in range(KC):
        if extra_scale is None:
            p.add("dve", lambda e, c=c: e.scalar_tensor_tensor(out=out[:, c, ocol0:ocol0 + N], in0=h[:, h_c0 + c, n0:n0 + N],
                                                               scalar=g_t[:, c:c + 1], in1=rstd[:, 0:N],
                                                               op0=ALU.mult, op1=ALU.mult),
                  reads=[hkey, rstdkey, gkey], writes=[okey])
        else:
            raise NotImplementedError


def dma_w(cx, eng, dst, dkey, src, semkey, reads=()):
    cx.p.add(eng, lambda e: e.dma_start(out=dst, in_=src, max_dma_last_dim=8192), reads=list(reads), writes=[dkey],
             dma=semkey)


class Cfg1:
    def __init__(self, D=4096, F=14336, NT=512, NTILES=4, FG=14, windows=(2, 4, 8, 16), eps=1e-6):
        self.D, self.F, self.NT, self.NTILES, self.FG = D, F, NT, NTILES, FG
        self.KC = D // 128
        self.FT = F // 128
        self.NG = self.FT // FG
        assert self.FT % FG == 0
        self.windows = windows
        self.GC = self.KC // len(windows)
        self.T = 16 + NT * NTILES
        self.eps = eps


def build_l1(cfg):
    nc = bass.Bass("TRN2", target_bir_lowering=False)
    D, F, NT, KC, FT, FG, NG, GC, T = cfg.D, cfg.F, cfg.NT, cfg.KC, cfg.FT, cfg.FG, cfg.NG, cfg.GC, cfg.T
    NW = len(cfg.windows)
    xT = nc.dram_tensor("xT", [128, KC, T], F32, kind="ExternalInput").ap()
    gmix = nc.dram_tensor("gmix", [128, KC], F32, kind="ExternalInput").ap()
    gffn = nc.dram_tensor("gffn", [128, KC], F32, kind="ExternalInput").ap()
    pscale = nc.dram_tensor("pscale", [128, KC], F32, kind="ExternalInput").ap()
    poolw = nc.dram_tensor("poolw", [NW, GC, 128, GC, 128], F32, kind="ExternalInput").ap()
    wg = nc.dram_tensor("wg", [FT, 128, KC, 128], F32, kind="ExternalInput").ap()
    wu = nc.dram_tensor("wu", [FT, 128, KC, 128], F32, kind="ExternalInput").ap()
    wd = nc.dram_tensor("wd", [NG, KC, 128, FG, 128], F32, kind="ExternalInput").ap()
    hT = nc.dram_tensor("hT", [128, KC, T], F32, kind="ExternalOutput").ap()

    from contextlib import ExitStack
    with ExitStack() as st:
        p = Prog(nc)
        cx = Ctx(nc, st, p)
        h = cx.sb([128, KC, NT], F32, "h")
        ub = cx.sb([128, KC, 16 + NT], BF16, "ub")
        carry = cx.sb([128, KC, 16], BF16, "carry")
        xb = cx.sb([128, max(FG, GC), NT], BF16, "xb")
        tmpA = cx.sb([128, 16 + NT], F32, "tmpA")
        tmpB = cx.sb([128, 16 + NT], F32, "tmpB")
        WS = 6
        wslot = [cx.sb([128, max(KC, FG), 128], BF16, "w%d" % i) for i in range(WS)]
        sq = [cx.sb([128, NT], F32, "sq%d" % i) for i in range(2)]
        rstd = cx.sb([128, NT], F32, "rstd")
        sg = [cx.sb([128, NT], F32, "sg%d" % i) for i in range(2)]
        g1 = cx.sb([128, KC], F32, "g1")
        g2 = cx.sb([128, KC], F32, "g2")
        psc = cx.sb([128, KC], F32, "psc")
        rc = cx.sb([128, NW, 16], F32, "rc")
        consts = emit_consts(cx, [1.0 / D])
        ones, oneskey = consts[1.0 / D]

        p.add("sp", lambda e: e.dma_start(out=g1[:], in_=gmix), writes=["g1"], dma="c1")
        p.add("sp", lambda e: e.dma_start(out=g2[:], in_=gffn), writes=["g2"], dma="c2")
        p.add("sp", lambda e: e.dma_start(out=psc[:], in_=pscale), writes=["psc"], dma="c3")
        for wi, w in enumerate(cfg.windows):
            p.add("pool", lambda e, wi=wi, w=w: e.memset(rc[:, wi, :], 1.0 / w), writes=["rc"])
            for t in range(min(w - 1, 16)):
                p.add("pool", lambda e, wi=wi, t=t: e.memset(rc[:, wi, t:t + 1], 1.0 / (t + 1)), writes=["rc"])
        p.add("pool", lambda e: e.memset(carry[:], 0.0), writes=["carry"])

        wi_ctr = [0]

        def next_w():
            i = wi_ctr[0] % WS
            wi_ctr[0] += 1
            return wslot[i], ("w", i), "w%d" % i

        tiles = [(0, 16)] + [(16 + i * NT, NT) for i in range(cfg.NTILES)]
        for ti, (n0, N) in enumerate(tiles):
            first = (ti == 0)
            p.add("sp", lambda e, n0=n0, N=N: e.dma_start(out=h[:, :, 0:N], in_=xT[:, :, n0:n0 + N]),
                  writes=["h"], dma="h")
            p.add("pool", lambda e: e.tensor_copy(out=ub[:, :, 0:16], in_=carry[:]), reads=["carry"], writes=["ub"])
            emit_rmsnorm(cx, h, "h", KC, 0, N, g1, "g1", ones, oneskey, cfg.eps, ub, "ub", 16,
                         sq, ["sq0", "sq1"], rstd, "rstd")
            p.add("pool", lambda e, N=N: e.tensor_copy(out=carry[:], in_=ub[:, :, N:N + 16]), reads=["ub"], writes=["carry"])
            for gi, w in enumerate(cfg.windows):
                nsteps = int(math.log2(w))
                for cl in range(GC):
                    c = gi * GC + cl
                    src = None
                    lo = 0
                    bufs = [tmpA, tmpB]
                    keys = ["tmpA", "tmpB"]
                    for s_ in range(nsteps):
                        sh = 1 << s_
                        dst = bufs[s_ % 2]
                        dk = keys[s_ % 2]
                        nlo = lo + sh
                        if src is None:
                            p.add("pool", lambda e, dst=dst, c=c, nlo=nlo, sh=sh, N=N: e.tensor_tensor(
                                out=dst[:, nlo:16 + N], in0=ub[:, c, nlo:16 + N], in1=ub[:, c, nlo - sh:16 + N - sh], op=ALU.add),
                                reads=["ub"], writes=[dk])
                        else:
                            sk = keys[(s_ - 1) % 2]
                            p.add("pool", lambda e, dst=dst, src=src, nlo=nlo, sh=sh, N=N: e.tensor_tensor(
                                out=dst[:, nlo:16 + N], in0=src[:, nlo:16 + N], in1=src[:, nlo - sh:16 + N - sh], op=ALU.add),
                                reads=[sk], writes=[dk])
                        src = dst
                        lo = nlo
                    sk = keys[(nsteps - 1) % 2]
                    if first:
                        ok = keys[nsteps % 2]
                        o2 = bufs[nsteps % 2]
                        p.add("pool", lambda e, src=src, o2=o2, gi=gi, N=N: e.tensor_tensor(
                            out=o2[:, 16:16 + N], in0=src[:, 16:16 + N], in1=rc[:, gi, 0:N], op=ALU.mult),
                            reads=[sk, "rc"], writes=[ok])
                        p.add("pool", lambda e, o2=o2, cl=cl, c=c, N=N: e.tensor_tensor(
                            out=xb[:, cl, 0:N], in0=o2[:, 16:16 + N], in1=ub[:, c, 16:16 + N], op=ALU.subtract),
                            reads=[ok, "ub"], writes=["xb"])
                    else:
                        p.add("dve", lambda e, src=src, cl=cl, c=c, w=w, N=N: e.scalar_tensor_tensor(
                            out=xb[:, cl, 0:N], in0=src[:, 16:16 + N], scalar=1.0 / w, in1=ub[:, c, 16:16 + N],
                            op0=ALU.mult, op1=ALU.subtract), reads=[sk, "ub"], writes=["xb"])
                for dt in range(GC):
                    wt, wk, wsem = next_w()
                    dma_w(cx, "pool", wt[:, 0:GC, :], wk, poolw[gi, dt], wsem)
                    b = cx.bank()
                    ps = cx.ps[b]
                    for kc in range(GC):
                        p.add("pe", lambda e, ps=ps, wt=wt, kc=kc, N=N: e.matmul(ps[:, 0:N], wt[:, kc, :], xb[:, kc, 0:N],
                                                                                 start=(kc == 0), stop=(kc == GC - 1)),
                              reads=[wk, "xb"], writes=[("ps", b)])
                    c = gi * GC + dt
                    p.add("dve", lambda e, ps=ps, c=c, N=N: e.scalar_tensor_tensor(
                        out=h[:, c, 0:N], in0=ps[:, 0:N], scalar=psc[:, c:c + 1], in1=h[:, c, 0:N], op0=ALU.mult, op1=ALU.add),
                        reads=[("ps", b), "psc", "h"], writes=["h"])
            emit_rmsnorm(cx, h, "h", KC, 0, N, g2, "g2", ones, oneskey, cfg.eps, ub, "ub", 0,
                         sq, ["sq0", "sq1"], rstd, "rstd")
            emit_ffn(cx, cfg, ub, "ub", 0, N, xb, "xb", h, "h", wg, wu, wd, None, next_w, sg, FT, FG, KC)
            p.add("sp", lambda e, n0=n0, N=N: e.dma_start(out=hT[:, :, n0:n0 + N], in_=h[:, :, 0:N]),
                  reads=["h"], dma="out")
        p.emit(final_wait_keys=["out"])
    return nc


def emit_ffn(cx, cfg, u, ukey, ucol0, N, xb, xbkey, h, hkey, wg, wu, wd, colscale, next_w, sg, FT, FG, KC):
    p = cx.p
    NG = FT // FG
    for grp in range(NG):
        for fl in range(FG):
            ft = grp * FG + fl
            wgt, wgk, wgs = next_w()
            dma_w(cx, "pool", wgt[:, 0:KC, :], wgk, wg[ft], wgs)
            wut, wuk, wus = next_w()
            dma_w(cx, "pool", wut[:, 0:KC, :], wuk, wu[ft], wus)
            bg = cx.bank()
            bu = cx.bank()
            psg, psu = cx.ps[bg], cx.ps[bu]
            for kc in range(KC):
                p.add("pe", lambda e, psg=psg, wgt=wgt, kc=kc: e.matmul(psg[:, 0:N], wgt[:, kc, :], u[:, kc, ucol0:ucol0 + N],
                                                                        start=(kc == 0), stop=(kc == KC - 1)),
                      reads=[wgk, ukey], writes=[("ps", bg)])
            for kc in range(KC):
                p.add("pe", lambda e, psu=psu, wut=wut, kc=kc: e.matmul(psu[:, 0:N], wut[:, kc, :], u[:, kc, ucol0:ucol0 + N],
                                                                        start=(kc == 0), stop=(kc == KC - 1)),
                      reads=[wuk, ukey], writes=[("ps", bu)])
            s = sg[ft % 2]
            sk = "sg%d" % (ft % 2)
            p.add("act", lambda e, s=s, psg=psg: e.activation(out=s[:, 0:N], in_=psg[:, 0:N], func=AF.Silu),
                  reads=[("ps", bg)], writes=[sk])
            p.add("dve", lambda e, s=s, psu=psu, fl=fl: e.tensor_tensor(out=xb[:, fl, 0:N], in0=psu[:, 0:N], in1=s[:, 0:N],
                                                                        op=ALU.mult),
                  reads=[("ps", bu), sk], writes=[xbkey])
            if colscale is not None:
                cs, csk = colscale
                p.add("dve", lambda e, fl=fl, cs=cs: e.tensor_tensor(out=xb[:, fl, 0:N], in0=xb[:, fl, 0:N], in1=cs,
                                                                      op=ALU.mult),
                      reads=[xbkey, csk], writes=[xbkey])
        for dt in range(KC):
            wt, wk, wsem = next_w()
            dma_w(cx, "pool", wt[:, 0:FG, :], wk, wd[grp, dt], wsem)
            b = cx.bank()
            ps = cx.ps[b]
            for fl in range(FG):
                p.add("pe", lambda e, ps=ps, wt=wt, fl=fl: e.matmul(ps[:, 0:N], wt[:, fl, :], xb[:, fl, 0:N],
                                                                    start=(fl == 0), stop=(fl == FG - 1)),
                      reads=[wk, xbkey], writes=[("ps", b)])
            p.add("dve", lambda e, ps=ps, dt=dt: e.tensor_tensor(out=h[:, dt, 0:N], in0=ps[:, 0:N], in1=h[:, dt, 0:N], op=ALU.add),
                  reads=[("ps", b), hkey], writes=[hkey])


def vec_t(v):
    return np.ascontiguousarray(np.asarray(v, np.float32).reshape(-1, 128).T)


def w_tiles(W):
    K, Fd = W.shape
    return np.ascontiguousarray(np.asarray(W, np.float32).reshape(K // 128, 128, Fd // 128, 128).transpose(2, 1, 0, 3))


def wd_tiles(W, FG):
    Fd, D = W.shape
    NG = Fd // 128 // FG
    return np.ascontiguousarray(np.asarray(W, np.float32).reshape(NG, FG, 128, D // 128, 128).transpose(0, 3, 2, 1, 4))


def to_fm(x):
    T, D = x.shape
    return np.ascontiguousarray(np.asarray(x, np.float32).reshape(T, D // 128, 128).transpose(2, 1, 0))


def from_fm(xT):
    P, KC, T = xT.shape
    return np.ascontiguousarray(xT.transpose(2, 1, 0).reshape(T, KC * 128))


def run_l1(cfg, seqs, meta, norm_mix0, norm_ffn0, pool_w0, pool_scale0, wg0, wu0, wd0):
    nc = build_l1(cfg)
    GC = cfg.GC
    NW = len(cfg.windows)
    pw = np.asarray(pool_w0, np.float32)
    pwt = np.ascontiguousarray(pw.reshape(NW, GC, 128, GC, 128).transpose(0, 3, 2, 1, 4))
    shared = dict(gmix=vec_t(norm_mix0), gffn=vec_t(norm_ffn0), pscale=vec_t(pool_scale0), poolw=pwt,
                  wg=w_tiles(wg0), wu=w_tiles(wu0), wd=wd_tiles(wd0, cfg.FG))
    in_maps = [dict(shared, xT=to_fm(s)) for s in seqs]
    res = run_bass_kernel_spmd(nc, in_maps, core_ids=list(range(len(seqs))))
    return [from_fm(r["hT"]) for r in res.results]


class Cfg2:
    def __init__(self, D=4096, HPC=4, LQ=8192, NT=512, eps=1e-6, sub_eps=1e-5, lambda_init=0.0):
        self.D, self.HPC, self.LQ, self.NT = D, HPC, LQ, NT
        self.KC = D // 128
        self.L = 16 + LQ
        self.NB = LQ // 128
        self.NCH = LQ // NT
        self.eps, self.sub_eps, self.lambda_init = eps, sub_eps, lambda_init


def build_l2(cfg):
    nc = bass.Bass("TRN2", target_bir_lowering=False)
    D, HPC, LQ, NT, KC, L, NB, NCH = cfg.D, cfg.HPC, cfg.LQ, cfg.NT, cfg.KC, cfg.L, cfg.NB, cfg.NCH
    BPC = NT // 128
    hT = nc.dram_tensor("hT", [128, KC, L], F32, kind="ExternalInput").ap()
    gmix = nc.dram_tensor("gmix", [128, KC], F32, kind="ExternalInput").ap()
    wqkv = nc.dram_tensor("wqkv", [HPC, 6, 128, KC, 128], F32, kind="ExternalInput").ap()
    qkn = nc.dram_tensor("qkn", [128, 2], F32, kind="ExternalInput").ap()
    lvec = nc.dram_tensor("lvec", [128, 4], F32, kind="ExternalInput").ap()
    subl = nc.dram_tensor("subl", [128, 2], F32, kind="ExternalInput").ap()
    tri_d = nc.dram_tensor("tri", [128, 128], F32, kind="ExternalInput").ap()
    oT = nc.dram_tensor("oT", [HPC, 2, 128, LQ], F32, kind="ExternalOutput").ap()
    uT = nc.dram_tensor("uT_scratch", [128, KC, L], BF16).ap()

    from contextlib import ExitStack
    with ExitStack() as st:
        p = Prog(nc)
        cx = Ctx(nc, st, p)
        NA = 128
        h = cx.sb([128, KC, NA], F32, "h")
        ub = cx.sb([128, KC, NT], BF16, "ub")
        sq = [cx.sb([128, NT], F32, "sq%d" % i) for i in range(2)]
        rstd = cx.sb([128, NT], F32, "rstd")
        g1 = cx.sb([128, KC], F32, "g1")
        qk = cx.sb([128, 2], F32, "qk")
        lv = cx.sb([128, 4], F32, "lv")
        sl = cx.sb([128, 2], F32, "sl")
        tri = cx.sb([128, 128], BF16, "tri_sb")
        onesb = cx.sb([128, 128], BF16, "onesb")
        pr = cx.sb([128, 2], F32, "pr")
        ex = cx.sb([128, 2], F32, "ex")
        neglam = cx.sb([128, 1], F32, "neglam")
        consts = emit_consts(cx, [1.0 / D, 1.0 / 128, 1.0 / 256, 1.0])
        onesD, onesDk = consts[1.0 / D]
        ones128, ones128k = consts[1.0 / 128]
        ones256, ones256k = consts[1.0 / 256]
        ones1, ones1k = consts[1.0]
        p.add("sp", lambda e: e.dma_start(out=g1[:], in_=gmix), writes=["g1"], dma="c1")
        p.add("sp", lambda e: e.dma_start(out=qk[:], in_=qkn), writes=["qk"], dma="c2")
        p.add("sp", lambda e: e.dma_start(out=lv[:], in_=lvec), writes=["lv"], dma="c3")
        p.add("sp", lambda e: e.dma_start(out=sl[:], in_=subl), writes=["sl"], dma="c4")
        p.add("pool", lambda e: e.dma_start(out=tri[:], in_=tri_d), writes=["tri"], dma="c5")
        p.add("pool", lambda e: e.memset(onesb[:], 1.0), writes=["onesb"])
        p.add("dve", lambda e: e.tensor_scalar(out=qk[:, 0:1], in0=qk[:, 0:1], scalar1=float(128 ** -0.5), scalar2=None, op0=ALU.mult),
              reads=["qk"], writes=["qk"])
        p.add("dve", lambda e: e.tensor_scalar(out=sl[:], in0=sl[:], scalar1=float(1.0 - cfg.lambda_init), scalar2=None, op0=ALU.mult),
              reads=["sl"], writes=["sl"])
        p.add("dve", lambda e: e.tensor_tensor(out=pr[:, 0:1], in0=lv[:, 0:1], in1=lv[:, 1:2], op=ALU.mult), reads=["lv"], writes=["pr"])
        p.add("dve", lambda e: e.tensor_tensor(out=pr[:, 1:2], in0=lv[:, 2:3], in1=lv[:, 3:4], op=ALU.mult), reads=["lv"], writes=["pr"])
        b = cx.bank()
        psl = cx.ps[b]
        p.add("pe", lambda e: e.matmul(psl[:, 0:2], ones1[:, :], pr[:, 0:2], start=True, stop=True), reads=["pr", ones1k], writes=[("ps", b)])
        p.add("act", lambda e: e.activation(out=ex[:], in_=psl[:, 0:2], func=AF.Exp), reads=[("ps", b)], writes=["ex"])
        p.add("dve", lambda e: e.tensor_tensor(out=neglam[:], in0=ex[:, 1:2], in1=ex[:, 0:1], op=ALU.subtract), reads=["ex"], writes=["neglam"])
        p.add("dve", lambda e: e.tensor_scalar(out=neglam[:], in0=neglam[:], scalar1=float(-cfg.lambda_init), scalar2=None, op0=ALU.add),
              reads=["neglam"], writes=["neglam"])

        tiles = [(0, 16)] + [(16 + i * NT, NT) for i in range(NCH)]
        tilesA = [(0, 16)] + [(16 + i * NA, NA) for i in range(LQ // NA)]
        for ti, (n0, N) in enumerate(tilesA):
            p.add("sp", lambda e, n0=n0, N=N: e.dma_start(out=h[:, :, 0:N], in_=hT[:, :, n0:n0 + N]), writes=["h"], dma="h")
            emit_rmsnorm(cx, h, "h", KC, 0, N, g1, "g1", onesD, onesDk, cfg.eps, ub, "ub", 0, sq, ["sq0", "sq1"], rstd, "rstd")
            p.add("sp", lambda e, n0=n0, N=N: e.dma_start(out=uT[:, :, n0:n0 + N], in_=ub[:, :, 0:N]), reads=["ub"],
                  writes=["uT"], dma="us")

        KT = cx.sb([128, 2, L], BF16, "KT")
        V = cx.sb([128, NB + 1, 256], BF16, "V")
        wsl = [cx.sb([128, KC, 128], BF16, "wq%d" % i) for i in range(4)]
        qc = cx.sb([128, 2, NT], BF16, "qc")
        et = [cx.sb([128, NT], BF16, "et%d" % i) for i in range(3)]
        rden = cx.sb([128, NT], F32, "rden")
        o1n = cx.sb([128, 2, NT], F32, "o1n")
        otmp = cx.sb([128, 2, NT], F32, "otmp")
        ofin, oout = otmp, o1n
        cx.rot = [3, 4, 5, 6, 7]
        cx.ps_i = 0
        ACC = (0, 1, 2)

        def qknorm(ps, psb, N, gcol, out_ap, okey):
            s = sq[0]
            p.add("act", lambda e: e.activation(out=s[:, 0:N], in_=ps[:, 0:N], func=AF.Square), reads=[("ps", psb)], writes=["sq0"])
            b2 = cx.bank()
            ps2 = cx.ps[b2]
            p.add("pe", lambda e: e.matmul(ps2[:, 0:N], ones128[:, :], s[:, 0:N], start=True, stop=True), reads=["sq0", ones128k],
                  writes=[("ps", b2)])
            epst, epskey = get_eps(cx, cfg.eps)
            p.add("act", lambda e: e.activation(out=rstd[:, 0:N], in_=ps2[:, 0:N], func=AF.Sqrt, bias=epst[:, 0:1], scale=1.0),
                  reads=[("ps", b2), epskey], writes=["rstd"])
            p.add("dve", lambda e: e.reciprocal(out=rstd[:, 0:N], in_=rstd[:, 0:N]), reads=["rstd"], writes=["rstd"])
            p.add("dve", lambda e: e.scalar_tensor_tensor(out=out_ap, in0=ps[:, 0:N], scalar=qk[:, gcol:gcol + 1], in1=rstd[:, 0:N],
                                                          op0=ALU.mult, op1=ALU.mult),
                  reads=[("ps", psb), "rstd", "qk"], writes=[okey])

        for hh in range(HPC):
            for i, wi in enumerate((2, 3, 4, 5)):
                dma_w(cx, "pool", wsl[i][:], ("wq", i), wqkv[hh, wi], "wq%d" % i)
            for ti, (n0, N) in enumerate(tiles):
                p.add("sp", lambda e, n0=n0, N=N: e.dma_start(out=ub[:, :, 0:N], in_=uT[:, :, n0:n0 + N]), reads=["uT"],
                      writes=["ub"], dma="ul")
                for m in range(2):
                    b = cx.bank()
                    ps = cx.ps[b]
                    for kc in range(KC):
                        p.add("pe", lambda e, ps=ps, m=m, kc=kc, N=N: e.matmul(ps[:, 0:N], wsl[m][:, kc, :], ub[:, kc, 0:N],
                                                                               start=(kc == 0), stop=(kc == KC - 1)),
                              reads=[("wq", m), "ub"], writes=[("ps", b)])
                    qknorm(ps, b, N, 1, KT[:, m, n0:n0 + N], "KT")
                nblk = max(1, N // 128)
                for bi in range(nblk):
                    M = min(128, N)
                    blk = 0 if ti == 0 else 1 + (n0 - 16) // 128 + bi
                    b = cx.bank()
                    ps = cx.ps[b]
                    for half in range(2):
                        for kc in range(KC):
                            p.add("pe", lambda e, ps=ps, kc=kc, half=half, bi=bi, M=M: e.matmul(
                                ps[0:M, half * 128:(half + 1) * 128], ub[:, kc, bi * 128:bi * 128 + M], wsl[2 + half][:, kc, :],
                                start=(kc == 0), stop=(kc == KC - 1)),
                                reads=[("wq", 2 + half), "ub"], writes=[("ps", b)])
                    p.add("act", lambda e, ps=ps, blk=blk, M=M: e.copy(out=V[0:M, blk, :], in_=ps[0:M, 0:256]), reads=[("ps", b)],
                          writes=["V"])
            for i, wi in enumerate((0, 1)):
                dma_w(cx, "pool", wsl[i][:], ("wq", i), wqkv[hh, wi], "wq%d" % i)
            for c in range(NCH):
                q0 = 16 + c * NT
                p.add("sp", lambda e, q0=q0: e.dma_start(out=ub[:, :, 0:NT], in_=uT[:, :, q0:q0 + NT]), reads=["uT"],
                      writes=["ub"], dma="ul")
                for m in range(2):
                    b = cx.bank()
                    ps = cx.ps[b]
                    for kc in range(KC):
                        p.add("pe", lambda e, ps=ps, m=m, kc=kc: e.matmul(ps[:, 0:NT], wsl[m][:, kc, :], ub[:, kc, 0:NT],
                                                                          start=(kc == 0), stop=(kc == KC - 1)),
                              reads=[("wq", m), "ub"], writes=[("ps", b)])
                    qknorm(ps, b, NT, 0, qc[:, m, :], "qc")
                for m in range(2):
                    kts = [(0, 16, 0, 0, False)]
                    for kb in range(BPC * c):
                        kts.append((16 + kb * 128, 128, 1 + kb, 0, False))
                    for i in range(BPC):
                        kb = BPC * c + i
                        kts.append((16 + kb * 128, 128, 1 + kb, 128 * i, True))
                    SK = 2
                    nk = len(kts)

                    def emit_score(ki):
                        k0, M, blk, qlo, masked = kts[ki]
                        bs = cx.bank()
                        pss = cx.ps[bs]
                        e_t = et[ki % 3]
                        ek = "et%d" % (ki % 3)
                        p.add("pe", lambda e, pss=pss, m=m, k0=k0, M=M, qlo=qlo: e.matmul(
                            pss[0:M, qlo:NT], KT[:, m, k0:k0 + M], qc[:, m, qlo:NT], start=True, stop=True),
                            reads=["KT", "qc"], writes=[("ps", bs)])
                        p.add("act", lambda e, pss=pss, e_t=e_t, M=M, qlo=qlo: e.activation(out=e_t[0:M, qlo:NT], in_=pss[0:M, qlo:NT],
                                                                                            func=AF.Exp),
                              reads=[("ps", bs)], writes=[ek])
                        if masked:
                            p.add("pool", lambda e, e_t=e_t, qlo=qlo: e.tensor_tensor(out=e_t[:, qlo:qlo + 128], in0=e_t[:, qlo:qlo + 128],
                                                                                      in1=tri[:, :], op=ALU.mult),
                                  reads=[ek, "tri"], writes=[ek])

                    def emit_pv(ki):
                        k0, M, blk, qlo, masked = kts[ki]
                        e_t = et[ki % 3]
                        ek = "et%d" % (ki % 3)
                        first, last = (ki == 0), (ki == nk - 1)
                        for ec in range(2):
                            p.add("pe", lambda e, ec=ec, e_t=e_t, M=M, blk=blk, qlo=qlo, first=first, last=last: e.matmul(
                                cx.ps[ACC[ec]][:, qlo:NT], V[0:M, blk, ec * 128:(ec + 1) * 128], e_t[0:M, qlo:NT], start=first, stop=last),
                                reads=["V", ek], writes=[("ps", ACC[ec])])
                        p.add("pe", lambda e, e_t=e_t, M=M, qlo=qlo, first=first, last=last: e.matmul(
                            cx.ps[ACC[2]][:, qlo:NT], onesb[0:M, :], e_t[0:M, qlo:NT], start=first, stop=last),
                            reads=["onesb", ek], writes=[("ps", ACC[2])])

                    for step in range(nk + SK):
                        if step < nk:
                            emit_score(step)
                        if step >= SK:
                            emit_pv(step - SK)
                    p.add("dve", lambda e: e.reciprocal(out=rden[:, :], in_=cx.ps[ACC[2]][:, 0:NT]), reads=[("ps", ACC[2])], writes=["rden"])
                    dstt, dk = (o1n, "o1n") if m == 0 else (otmp, "otmp")
                    for ec in range(2):
                        p.add("dve", lambda e, ec=ec, dstt=dstt: e.tensor_tensor(out=dstt[:, ec, :], in0=cx.ps[ACC[ec]][:, 0:NT], in1=rden[:, :],
                                                                                 op=ALU.mult),
                              reads=[("ps", ACC[ec]), "rden"], writes=[dk])
                for ec in range(2):
                    p.add("dve", lambda e, ec=ec: e.scalar_tensor_tensor(out=ofin[:, ec, :], in0=otmp[:, ec, :], scalar=neglam[:, 0:1],
                                                                         in1=o1n[:, ec, :], op0=ALU.mult, op1=ALU.add),
                          reads=["otmp", "o1n", "neglam"], writes=["otmp"])
                bsn = cx.bank()
                psn = cx.ps[bsn]
                for ec in range(2):
                    s = sq[ec]
                    p.add("act", lambda e, s=s, ec=ec: e.activation(out=s[:, 0:NT], in_=ofin[:, ec, :], func=AF.Square), reads=["otmp"],
                          writes=["sq%d" % ec])
                    p.add("pe", lambda e, s=s, ec=ec: e.matmul(psn[:, 0:NT], ones256[:, :], s[:, 0:NT], start=(ec == 0), stop=(ec == 1)),
                          reads=["sq%d" % ec, ones256k], writes=[("ps", bsn)])
                epst, epskey = get_eps(cx, cfg.sub_eps)
                p.add("act", lambda e: e.activation(out=rstd[:, 0:NT], in_=psn[:, 0:NT], func=AF.Sqrt, bias=epst[:, 0:1], scale=1.0),
                      reads=[("ps", bsn), epskey], writes=["rstd"])
                p.add("dve", lambda e: e.reciprocal(out=rstd[:, 0:NT], in_=rstd[:, 0:NT]), reads=["rstd"], writes=["rstd"])
                for ec in range(2):
                    p.add("dve", lambda e, ec=ec: e.scalar_tensor_tensor(out=oout[:, ec, :], in0=ofin[:, ec, :], scalar=sl[:, ec:ec + 1],
                                                                         in1=rstd[:, 0:NT], op0=ALU.mult, op1=ALU.mult),
                          reads=["otmp", "rstd", "sl"], writes=["o1n"])
                    p.add("sp", lambda e, ec=ec, hh=hh, c=c: e.dma_start(out=oT[hh, ec, :, c * NT:(c + 1) * NT], in_=oout[:, ec, :]),
                          reads=["o1n"], dma="out")
        p.emit(final_wait_keys=["out"])
    return nc


def qkv_tiles(w_qkv, heads, n_heads):
    W = np.asarray(w_qkv, np.float32)
    D = W.shape[0]
    KC = D // 128
    DQK = n_heads * 256
    out = np.empty((len(heads), 6, 128, KC, 128), np.float32)
    for i, hd in enumerate(heads):
        cols = [hd * 256, hd * 256 + 128, DQK + hd * 256, DQK + hd * 256 + 128, 2 * DQK + hd * 256, 2 * DQK + hd * 256 + 128]
        for j, c0 in enumerate(cols):
            out[i, j] = W[:, c0:c0 + 128].reshape(KC, 128, 128).transpose(1, 0, 2)
    return out


def run_l2(cfg, h1_list, norm_mix1, w_qkv, q_norm, k_norm, lq1, lk1, lq2, lk2, subln, n_heads, head_groups=None):
    nc = build_l2(cfg)
    HPC = cfg.HPC
    ngrp = n_heads // HPC
    tri = np.triu(np.ones((128, 128), np.float32))
    shared = dict(gmix=vec_t(norm_mix1),
                  qkn=np.ascontiguousarray(np.stack([q_norm, k_norm], axis=1).astype(np.float32)),
                  lvec=np.ascontiguousarray(np.stack([lq1, lk1, lq2, lk2], axis=1).astype(np.float32)),
                  subl=np.ascontiguousarray(np.asarray(subln, np.float32).reshape(2, 128).T), tri=tri)
    in_maps = []
    hfm = [to_fm(hb) for hb in h1_list]
    wts = [qkv_tiles(w_qkv, list(range(g * HPC, (g + 1) * HPC)), n_heads) for g in range(ngrp)]
    for b in range(len(h1_list)):
        for g in range(ngrp):
            in_maps.append(dict(shared, hT=hfm[b], wqkv=wts[g]))
    res = run_bass_kernel_spmd(nc, in_maps, core_ids=list(range(len(in_maps))))
    outs = []
    i = 0
    for b in range(len(h1_list)):
        parts = []
        for g in range(ngrp):
            o = res.results[i]["oT"]
            i += 1
            parts.append(o.transpose(3, 0, 1, 2).reshape(cfg.LQ, HPC * 256))
        outs.append(np.concatenate(parts, axis=1))
    return outs


class Cfg3:
    def __init__(self, D=4096, FE=3584, NE=8, NT=512, NTILES=4, FG=14, eps=1e-6):
        self.D, self.FE, self.NE, self.NT, self.NTILES, self.FG = D, FE, NE, NT, NTILES, FG
        self.KC = D // 128
        self.FT = FE // 128
        assert self.FT % FG == 0
        self.NG = self.FT // FG
        self.T = NT * NTILES
        self.eps = eps


def build_l3(cfg):
    nc = bass.Bass("TRN2", target_bir_lowering=False)
    D, FE, NE, NT, KC, FT, FG, NG, T = cfg.D, cfg.FE, cfg.NE, cfg.NT, cfg.KC, cfg.FT, cfg.FG, cfg.NG, cfg.T
    hT = nc.dram_tensor("hT", [128, KC, T], F32, kind="ExternalInput").ap()
    oT = nc.dram_tensor("oT", [128, KC, T], F32, kind="ExternalInput").ap()
    gffn = nc.dram_tensor("gffn", [128, KC], F32, kind="ExternalInput").ap()
    wo = nc.dram_tensor("wo", [KC, 128, KC, 128], F32, kind="ExternalInput").ap()
    rt = nc.dram_tensor("rt", [128, KC, NE], F32, kind="ExternalInput").ap()
    ident_d = nc.dram_tensor("ident", [128, 128], F32, kind="ExternalInput").ap()
    weg = nc.dram_tensor("weg", [NE, FT, 128, KC, 128], F32, kind="ExternalInput").ap()
    weu = nc.dram_tensor("weu", [NE, FT, 128, KC, 128], F32, kind="ExternalInput").ap()
    wed = nc.dram_tensor("wed", [NE, NG, KC, 128, FG, 128], F32, kind="ExternalInput").ap()
    outT = nc.dram_tensor("outT", [128, KC, T], F32, kind="ExternalOutput").ap()
    dbg = nc.dram_tensor("dbg", [128, NE, T], F32, kind="ExternalOutput").ap() if getattr(cfg, "debug", False) else None

    from contextlib import ExitStack
    with ExitStack() as st:
        p = Prog(nc)
        cx = Ctx(nc, st, p)
        h = cx.sb([128, KC, NT], F32, "h")
        ub = cx.sb([128, KC, NT], BF16, "ub")
        xb = cx.sb([128, FG, NT], BF16, "xb")
        WS = 6
        wslot = [cx.sb([128, max(KC, FG), 128], BF16, "w%d" % i) for i in range(WS)]
        sq = [cx.sb([128, NT], F32, "sq%d" % i) for i in range(2)]
        rstd = cx.sb([128, NT], F32, "rstd")
        sg = [cx.sb([128, NT], F32, "sg%d" % i) for i in range(2)]
        g2 = cx.sb([128, KC], F32, "g2")
        rtb = cx.sb([128, KC, NE], F32, "rtb")
        uf = [cx.sb([128, 128], F32, "uf%d" % i) for i in range(2)]
        ident = cx.sb([128, 128], F32, "ident_sb")
        combb = cx.sb([128, NE, NT], F32, "combb")
        lg = cx.sb([128, NE], F32, "lg")
        lg2 = cx.sb([128, NE], F32, "lg2")
        eq1 = cx.sb([128, NE], F32, "eq1")
        eq2 = cx.sb([128, NE], F32, "eq2")
        comb_all = cx.sb([128, NT // 128, NE], F32, "comb_all")
        sm = cx.sb([128, 8], F32, "sm")
        diag = [cx.sb([128, 128], F32, "diag%d" % i) for i in range(2)]
        consts = emit_consts(cx, [1.0 / D, 1.0])
        onesD, onesDk = consts[1.0 / D]
        ones1, ones1k = consts[1.0]
        p.add("sp", lambda e: e.dma_start(out=g2[:], in_=gffn), writes=["g2"], dma="c1")
        p.add("sp", lambda e: e.dma_start(out=ident[:], in_=ident_d), writes=["ident"], dma="c2")
        p.add("sp", lambda e: e.dma_start(out=rtb[:], in_=rt), writes=["rtb"], dma="c3")
        wi_ctr = [0]

        def next_w():
            i = wi_ctr[0] % WS
            wi_ctr[0] += 1
            return wslot[i], ("w", i), "w%d" % i

        AX = mybir.AxisListType.X
        for ti in range(cfg.NTILES):
            n0 = ti * NT
            N = NT
            p.add("sp", lambda e, n0=n0: e.dma_start(out=h[:, :, :], in_=hT[:, :, n0:n0 + NT]), writes=["h"], dma="h")
            for half in range(2):
                c0, c1 = half * KC // 2, (half + 1) * KC // 2
                p.add("pool", lambda e, n0=n0, c0=c0, c1=c1: e.dma_start(out=ub[:, c0:c1, :], in_=oT[:, c0:c1, n0:n0 + NT],
                                                                         max_dma_last_dim=8192),
                      writes=["ub"], dma="o")
            for dt in range(KC):
                wt, wk, wsem = next_w()
                dma_w(cx, "pool", wt[:, 0:KC, :], wk, wo[dt], wsem)
                b = cx.bank()
                ps = cx.ps[b]
                for kc in range(KC):
                    p.add("pe", lambda e, ps=ps, wt=wt, kc=kc: e.matmul(ps[:, 0:N], wt[:, kc, :], ub[:, kc, 0:N],
                                                                        start=(kc == 0), stop=(kc == KC - 1)),
                          reads=[wk, "ub"], writes=[("ps", b)])
                p.add("dve", lambda e, ps=ps, dt=dt: e.tensor_tensor(out=h[:, dt, 0:N], in0=ps[:, 0:N], in1=h[:, dt, 0:N], op=ALU.add),
                      reads=[("ps", b), "h"], writes=["h"])
            emit_rmsnorm(cx, h, "h", KC, 0, N, g2, "g2", onesD, onesDk, cfg.eps, ub, "ub", 0, sq, ["sq0", "sq1"], rstd, "rstd")
            cx.rot = [0, 1, 2, 3]
            cb = [4, 5, 6, 7]
            for tb in range(NT // 128):
                b = cx.bank()
                ps = cx.ps[b]
                for kc in range(KC):
                    uft = uf[kc % 2]
                    ufk = "uf%d" % (kc % 2)
                    p.add("dve", lambda e, uft=uft, kc=kc, tb=tb: e.scalar_tensor_tensor(
                        out=uft[:, :], in0=h[:, kc, tb * 128:(tb + 1) * 128], scalar=g2[:, kc:kc + 1],
                        in1=rstd[:, tb * 128:(tb + 1) * 128], op0=ALU.mult, op1=ALU.mult),
                        reads=["h", "g2", "rstd"], writes=[ufk])
                    p.add("pe", lambda e, ps=ps, kc=kc, uft=uft: e.matmul(ps[:, 0:NE], uft[:, :], rtb[:, kc, :],
                                                                          start=(kc == 0), stop=(kc == KC - 1)),
                          reads=[ufk, "rtb"], writes=[("ps", b)])
                p.add("dve", lambda e, ps=ps: e.tensor_copy(out=lg[:], in_=ps[:, 0:NE]), reads=[("ps", b)], writes=["lg"])
                p.add("dve", lambda e: e.tensor_reduce(out=sm[:, 0:1], in_=lg[:], axis=AX, op=ALU.max), reads=["lg"], writes=["sm"])
                p.add("dve", lambda e: e.tensor_scalar(out=eq1[:], in0=lg[:], scalar1=sm[:, 0:1], scalar2=None, op0=ALU.is_equal),
                      reads=["lg", "sm"], writes=["eq1"])
                p.add("dve", lambda e: e.scalar_tensor_tensor(out=lg2[:], in0=eq1[:], scalar=-1e30, in1=lg[:], op0=ALU.mult, op1=ALU.add),
                      reads=["eq1", "lg"], writes=["lg2"])
                p.add("dve", lambda e: e.tensor_reduce(out=sm[:, 1:2], in_=lg2[:], axis=AX, op=ALU.max), reads=["lg2"], writes=["sm"])
                p.add("dve", lambda e: e.tensor_scalar(out=eq2[:], in0=lg2[:], scalar1=sm[:, 1:2], scalar2=None, op0=ALU.is_equal),
                      reads=["lg2", "sm"], writes=["eq2"])
                p.add("dve", lambda e: e.tensor_tensor(out=sm[:, 2:3], in0=sm[:, 1:2], in1=sm[:, 0:1], op=ALU.subtract), reads=["sm"], writes=["sm"])
                p.add("act", lambda e: e.activation(out=sm[:, 3:4], in_=sm[:, 2:3], func=AF.Exp), reads=["sm"], writes=["sm"])
                p.add("dve", lambda e: e.tensor_scalar(out=sm[:, 4:5], in0=sm[:, 3:4], scalar1=1.0, scalar2=None, op0=ALU.add), reads=["sm"], writes=["sm"])
                p.add("dve", lambda e: e.reciprocal(out=sm[:, 5:6], in_=sm[:, 4:5]), reads=["sm"], writes=["sm"])
                p.add("dve", lambda e: e.tensor_tensor(out=sm[:, 6:7], in0=sm[:, 3:4], in1=sm[:, 5:6], op=ALU.mult), reads=["sm"], writes=["sm"])
                p.add("dve", lambda e, tb=tb: e.tensor_scalar(out=comb_all[:, tb, :], in0=eq1[:], scalar1=sm[:, 5:6], scalar2=None, op0=ALU.mult),
                      reads=["eq1", "sm"], writes=["comb"])
                p.add("dve", lambda e, tb=tb: e.scalar_tensor_tensor(out=comb_all[:, tb, :], in0=eq2[:], scalar=sm[:, 6:7], in1=comb_all[:, tb, :], op0=ALU.mult, op1=ALU.add),
                      reads=["eq2", "sm", "comb"], writes=["comb"])
            for eh in range(0, NE, 4):
                for ex_ in range(eh, min(NE, eh + 4)):
                    for tb in range(NT // 128):
                        dg = diag[(ex_ * (NT // 128) + tb) % 2]
                        dk = "diag%d" % ((ex_ * (NT // 128) + tb) % 2)
                        p.add("dve", lambda e, dg=dg, ex_=ex_, tb=tb: e.tensor_scalar(out=dg[:], in0=ident[:], scalar1=comb_all[:, tb, ex_:ex_ + 1],
                                                                                      scalar2=None, op0=ALU.mult),
                              reads=["ident", "comb"], writes=[dk])
                        p.add("pe", lambda e, dg=dg, ex_=ex_, tb=tb: e.matmul(cx.ps[cb[ex_ % 4]][:, tb * 128:(tb + 1) * 128], ones1[:, :], dg[:, :],
                                                                              start=True, stop=True),
                              reads=[dk, ones1k], writes=[("ps", cb[ex_ % 4])])
                    p.add("act", lambda e, ex_=ex_: e.copy(out=combb[:, ex_, :], in_=cx.ps[cb[ex_ % 4]][:, 0:NT]), reads=[("ps", cb[ex_ % 4])],
                          writes=[("combb", ex_)])
            cx.rot = None
            if dbg is not None:
                p.add("sp", lambda e, n0=n0: e.dma_start(out=dbg[:, :, n0:n0 + NT], in_=combb[:, :, :]),
                      reads=[("combb", i) for i in range(NE)], dma="out")
            for ex_ in range(NE):
                emit_ffn(cx, cfg, ub, "ub", 0, N, xb, "xb", h, "h", weg[ex_], weu[ex_], wed[ex_], (combb[:, ex_, :], ("combb", ex_)),
                         next_w, sg, FT, FG, KC)
            p.add("sp", lambda e, n0=n0: e.dma_start(out=outT[:, :, n0:n0 + NT], in_=h[:, :, :]), reads=["h"], dma="out")
        p.emit(final_wait_keys=["out"])
    return nc


def run_l3(cfg, h_list, o_list, norm_ffn1, w_o, router, eg, eu, ed):
    nc = build_l3(cfg)
    NE = cfg.NE
    KC = cfg.KC
    shared = dict(gffn=vec_t(norm_ffn1), wo=w_tiles(w_o),
                  rt=np.ascontiguousarray(np.asarray(router, np.float32).reshape(KC, 128, NE).transpose(1, 0, 2)),
                  ident=np.eye(128, dtype=np.float32),
                  weg=np.stack([w_tiles(eg[e]) for e in range(NE)]),
                  weu=np.stack([w_tiles(eu[e]) for e in range(NE)]),
                  wed=np.stack([wd_tiles(ed[e], cfg.FG) for e in range(NE)]))
    in_maps = [dict(shared, hT=to_fm(hh), oT=to_fm(oo)) for hh, oo in zip(h_list, o_list)]
    res = run_bass_kernel_spmd(nc, in_maps, core_ids=list(range(len(in_maps))))
    if getattr(cfg, "debug", False):
        return [from_fm(r["outT"]) for r in res.results], [r["dbg"] for r in res.results]
    return [from_fm(r["outT"]) for r in res.results]


def kernel(x, meta_tokens, norm_mix, norm_ffn, pool_w, pool_scale, ffn_w_gate, ffn_w_up, ffn_w_down,
           w_qkv, q_norm, k_norm, lambda_q1, lambda_k1, lambda_q2, lambda_k2, subln, w_o,
           router, exp_w_gate, exp_w_up, exp_w_down):
    x = np.asarray(x, np.float32)
    meta = np.asarray(meta_tokens, np.float32)
    B, S, D = x.shape
    NG = N_CORES // B
    per = S // NG
    full = [np.concatenate([meta, x[b]], axis=0) for b in range(B)]
    cfg1 = Cfg1(D=D, F=np.asarray(ffn_w_gate).shape[-1], NT=512, NTILES=per // 512, FG=14)
    seqs = [full[b][per * j: per * j + 16 + per] for b in range(B) for j in range(NG)]
    p1 = run_l1(cfg1, seqs, meta, np.asarray(norm_mix)[0], np.asarray(norm_ffn)[0], np.asarray(pool_w)[0],
                np.asarray(pool_scale)[0], np.asarray(ffn_w_gate)[0], np.asarray(ffn_w_up)[0], np.asarray(ffn_w_down)[0])
    h1 = [np.concatenate([p1[NG * b][0:16]] + [p1[NG * b + j][16:] for j in range(NG)], axis=0) for b in range(B)]
    del p1, seqs, full
    n_heads = 16
    cfg2 = Cfg2(D=D, HPC=n_heads // NG, LQ=S, NT=512, lambda_init=0.8 - 0.6 * math.exp(-0.3 * 1))
    o = run_l2(cfg2, h1, np.asarray(norm_mix)[1], np.asarray(w_qkv)[0], np.asarray(q_norm)[0], np.asarray(k_norm)[0],
               np.asarray(lambda_q1)[0], np.asarray(lambda_k1)[0], np.asarray(lambda_q2)[0], np.asarray(lambda_k2)[0],
               np.asarray(subln)[0], n_heads=n_heads)
    cfg3 = Cfg3(D=D, FE=np.asarray(exp_w_gate).shape[-1], NE=np.asarray(exp_w_gate).shape[1], NT=512, NTILES=per // 512, FG=14)
    h_list = [h1[b][16 + per * j: 16 + per * (j + 1)] for b in range(B) for j in range(NG)]
    o_list = [o[b][per * j: per * (j + 1)] for b in range(B) for j in range(NG)]
    outs = run_l3(cfg3, h_list, o_list, np.asarray(norm_ffn)[1], np.asarray(w_o)[0], np.asarray(router)[0],
                  np.asarray(exp_w_gate)[0], np.asarray(exp_w_up)[0], np.asarray(exp_w_down)[0])
    out = np.stack([np.concatenate(outs[NG * b: NG * (b + 1)], axis=0) for b in range(B)])
    return np.ascontiguousarray(out.astype(np.float32))
```

```python
import math
import numpy as np
import concourse.bass as bass
import concourse.mybir as mybir
from concourse.bass_utils import run_bass_kernel_spmd

F32 = mybir.dt.float32
BF16 = mybir.dt.bfloat16
AF = mybir.ActivationFunctionType
ALU = mybir.AluOpType

N_CORES = 8


class Prog:
    def __init__(self, nc, same_engine_sync=True):
        self.nc = nc
        self.ins = []
        self.last_writer = {}
        self.readers = {}
        self.same_engine_sync = same_engine_sync

    def add(self, eng, fn, reads=(), writes=(), dma=None):
        idx = len(self.ins)
        deps = set()
        for b in reads:
            w = self.last_writer.get(b)
            if w is not None:
                deps.add(w)
        for b in writes:
            w = self.last_writer.get(b)
            if w is not None:
                deps.add(w)
            deps.update(self.readers.get(b, ()))
        deps.discard(idx)
        fdeps = []
        for d in deps:
            p = self.ins[d]
            if p["dma"] is None and p["eng"] == eng:
                if eng == "pe" or not self.same_engine_sync:
                    continue
            fdeps.append(d)
        self.ins.append(dict(eng=eng, fn=fn, deps=sorted(fdeps), dma=dma))
        for b in reads:
            self.readers.setdefault(b, []).append(idx)
        for b in writes:
            self.last_writer[b] = idx
            self.readers[b] = []
        return idx

    def emit(self, final_wait_keys=()):
        nc = self.nc
        needed = set()
        for it in self.ins:
            needed.update(it["deps"])
        semkeys = []
        for it in self.ins:
            k = ("dma", it["dma"]) if it["dma"] is not None else ("eng", it["eng"])
            if k not in semkeys:
                semkeys.append(k)
        engs = ["pe", "act", "dve", "pool", "sp"]
        eng_obj = dict(pe=nc.tensor, act=nc.scalar, dve=nc.vector, pool=nc.gpsimd, sp=nc.sync)
        from contextlib import ExitStack
        with ExitStack() as st:
            sems = {}
            for i, k in enumerate(semkeys):
                sems[k] = st.enter_context(nc.semaphore("s%d" % i))
            cnt = {k: 0 for k in semkeys}
            ev = {}
            for idx, it in enumerate(self.ins):
                if it["dma"] is not None:
                    k = ("dma", it["dma"])
                    cnt[k] += 16
                    ev[idx] = (k, cnt[k], 16)
                elif idx in needed:
                    k = ("eng", it["eng"])
                    cnt[k] += 1
                    ev[idx] = (k, cnt[k], 1)
            streams = {e: [] for e in engs}
            waited = {e: {} for e in engs}
            for idx, it in enumerate(self.ins):
                e = it["eng"]
                waits = []
                for d in it["deps"]:
                    k, v, _ = ev[d]
                    if waited[e].get(k, 0) >= v:
                        continue
                    waited[e][k] = v
                    waits.append((k, v))
                streams[e].append((waits, it["fn"], ev.get(idx)))
            finals = [(("dma", k), cnt[("dma", k)]) for k in final_wait_keys if ("dma", k) in cnt]
            block = st.enter_context(nc.Block())

            def run(e):
                eo = eng_obj[e]
                for waits, fn, evv in streams[e]:
                    for k, v in waits:
                        eo.wait_ge(sems[k], v)
                    ins = fn(eo)
                    if evv is not None:
                        ins.then_inc(sems[evv[0]], evv[2])
                if e == "sp":
                    for k, v in finals:
                        eo.wait_ge(sems[k], v)

            @block.tensor
            def _(x):
                run("pe")

            @block.scalar
            def _(x):
                run("act")

            @block.vector
            def _(x):
                run("dve")

            @block.gpsimd
            def _(x):
                run("pool")

            @block.sync
            def _(x):
                run("sp")


class Ctx:
    def __init__(self, nc, st, prog):
        self.nc, self.st, self.p = nc, st, prog
        self.ps = [st.enter_context(nc.psum_tensor("ps%d" % i, [128, 512], F32)) for i in range(8)]
        self.ps_i = 0
        self.n = 0

    def sb(self, shape, dt, name=None):
        self.n += 1
        return self.st.enter_context(self.nc.sbuf_tensor(name or ("t%d" % self.n), shape, dt))

    def bank(self):
        rot = getattr(self, "rot", None) or list(range(8))
        i = rot[self.ps_i % len(rot)]
        self.ps_i += 1
        return i


def emit_consts(cx, inv_d_list):
    p = cx.p
    out = {}
    for v in inv_d_list:
        t = cx.sb([128, 128], F32)
        p.add("pool", lambda e, t=t, v=v: e.memset(t[:], float(v)), writes=[("c", id(t))])
        out[v] = (t, ("c", id(t)))
    return out


def get_eps(cx, eps):
    d = cx.__dict__.setdefault("_eps", {})
    if eps not in d:
        t = cx.sb([128, 1], F32)
        cx.p.add("pool", lambda e: e.memset(t[:], float(eps)), writes=[("eps", eps)])
        d[eps] = (t, ("eps", eps))
    return d[eps]


def emit_rmsnorm(cx, h, hkey, KC, n0, N, g_t, gkey, ones, oneskey, eps, out, okey, ocol0, sq, sqkeys, rstd, rstdkey,
                 h_c0=0, extra_scale=None):
    p = cx.p
    b = cx.bank()
    ps = cx.ps[b]
    G = max(1, min(KC, int(sq[0].shape[-1]) // N))
    while KC % G:
        G -= 1
    for gi, c0 in enumerate(range(0, KC, G)):
        s = sq[gi % 2]
        sk = sqkeys[gi % 2]
        if G == 1:
            p.add("act", lambda e, s=s, c=c0: e.activation(out=s[:, 0:N], in_=h[:, h_c0 + c, n0:n0 + N], func=AF.Square),
                  reads=[hkey], writes=[sk])
        else:
            p.add("act", lambda e, s=s, c=c0: e.activation(out=s[:, 0:G * N].rearrange("p (g n) -> p g n", g=G),
                                                           in_=h[:, h_c0 + c:h_c0 + c + G, n0:n0 + N], func=AF.Square),
                  reads=[hkey], writes=[sk])
        for j in range(G):
            c = c0 + j
            p.add("pe", lambda e, s=s, c=c, j=j: e.matmul(ps[:, 0:N], ones[:, :], s[:, j * N:(j + 1) * N], start=(c == 0), stop=(c == KC - 1)),
                  reads=[sk, oneskey], writes=[("ps", b)])
    epst, epskey = get_eps(cx, eps)
    p.add("act", lambda e: e.activation(out=rstd[:, 0:N], in_=ps[:, 0:N], func=AF.Sqrt, bias=epst[:, 0:1], scale=1.0),
          reads=[("ps", b), epskey], writes=[rstdkey])
    p.add("dve", lambda e: e.reciprocal(out=rstd[:, 0:N], in_=rstd[:, 0:N]), reads=[rstdkey], writes=[rstdkey])
    for c in range(KC):
        if extra_scale is None:
            p.add("dve", lambda e, c=c: e.scalar_tensor_tensor(out=out[:, c, ocol0:ocol0 + N], in0=h[:, h_c0 + c, n0:n0 + N],
                                                               scalar=g_t[:, c:c + 1], in1=rstd[:, 0:N],
                                                               op0=ALU.mult, op1=ALU.mult),
                  reads=[hkey, rstdkey, gkey], writes=[okey])
        else:
            raise NotImplementedError


def dma_w(cx, eng, dst, dkey, src, semkey, reads=()):
    cx.p.add(eng, lambda e: e.dma_start(out=dst, in_=src, max_dma_last_dim=8192), reads=list(reads), writes=[dkey],
             dma=semkey)


class Cfg1:
    def __init__(self, D=4096, F=14336, NT=512, NTILES=4, FG=14, windows=(2, 4, 8, 16), eps=1e-6):
        self.D, self.F, self.NT, self.NTILES, self.FG = D, F, NT, NTILES, FG
        self.KC = D // 128
        self.FT = F // 128
        self.NG = self.FT // FG
        assert self.FT % FG == 0
        self.windows = windows
        self.GC = self.KC // len(windows)
        self.T = 16 + NT * NTILES
        self.eps = eps


def build_l1(cfg):
    nc = bass.Bass("TRN2", target_bir_lowering=False)
    D, F, NT, KC, FT, FG, NG, GC, T = cfg.D, cfg.F, cfg.NT, cfg.KC, cfg.FT, cfg.FG, cfg.NG, cfg.GC, cfg.T
    NW = len(cfg.windows)
    xT = nc.dram_tensor("xT", [128, KC, T], F32, kind="ExternalInput").ap()
    gmix = nc.dram_tensor("gmix", [128, KC], F32, kind="ExternalInput").ap()
    gffn = nc.dram_tensor("gffn", [128, KC], F32, kind="ExternalInput").ap()
    pscale = nc.dram_tensor("pscale", [128, KC], F32, kind="ExternalInput").ap()
    poolw = nc.dram_tensor("poolw", [NW, GC, 128, GC, 128], F32, kind="ExternalInput").ap()
    wg = nc.dram_tensor("wg", [FT, 128, KC, 128], F32, kind="ExternalInput").ap()
    wu = nc.dram_tensor("wu", [FT, 128, KC, 128], F32, kind="ExternalInput").ap()
    wd = nc.dram_tensor("wd", [NG, KC, 128, FG, 128], F32, kind="ExternalInput").ap()
    hT = nc.dram_tensor("hT", [128, KC, T], F32, kind="ExternalOutput").ap()
    gnext = nc.dram_tensor("gnext", [128, KC], F32, kind="ExternalInput").ap()
    uTo = nc.dram_tensor("uTo", [128, KC, T], BF16, kind="ExternalOutput").ap()

    from contextlib import ExitStack
    with ExitStack() as st:
        p = Prog(nc)
        cx = Ctx(nc, st, p)
        h = cx.sb([128, KC, NT], F32, "h")
        ub = cx.sb([128, KC, 16 + NT], BF16, "ub")
        g3 = cx.sb([128, KC], F32, "g3")
        p.add("sp", lambda e: e.dma_start(out=g3[:], in_=gnext), writes=["g3"], dma="c4")
        carry = cx.sb([128, KC, 16], BF16, "carry")
        xb = cx.sb([128, max(FG, GC), NT], BF16, "xb")
        tmpA = cx.sb([128, 16 + NT], F32, "tmpA")
        tmpB = cx.sb([128, 16 + NT], F32, "tmpB")
        WS = 6
        wslot = [cx.sb([128, max(KC, FG), 128], BF16, "w%d" % i) for i in range(WS)]
        sq = [cx.sb([128, NT], F32, "sq%d" % i) for i in range(2)]
        rstd = cx.sb([128, NT], F32, "rstd")
        sg = [cx.sb([128, NT], F32, "sg%d" % i) for i in range(2)]
        g1 = cx.sb([128, KC], F32, "g1")
        g2 = cx.sb([128, KC], F32, "g2")
        psc = cx.sb([128, KC], F32, "psc")
        rc = cx.sb([128, NW, 16], F32, "rc")
        consts = emit_consts(cx, [1.0 / D])
        ones, oneskey = consts[1.0 / D]

        p.add("sp", lambda e: e.dma_start(out=g1[:], in_=gmix), writes=["g1"], dma="c1")
        p.add("sp", lambda e: e.dma_start(out=g2[:], in_=gffn), writes=["g2"], dma="c2")
        p.add("sp", lambda e: e.dma_start(out=psc[:], in_=pscale), writes=["psc"], dma="c3")
        for wi, w in enumerate(cfg.windows):
            p.add("pool", lambda e, wi=wi, w=w: e.memset(rc[:, wi, :], 1.0 / w), writes=["rc"])
            for t in range(min(w - 1, 16)):
                p.add("pool", lambda e, wi=wi, t=t: e.memset(rc[:, wi, t:t + 1], 1.0 / (t + 1)), writes=["rc"])
        p.add("pool", lambda e: e.memset(carry[:], 0.0), writes=["carry"])

        wi_ctr = [0]

        def next_w():
            i = wi_ctr[0] % WS
            wi_ctr[0] += 1
            return wslot[i], ("w", i), "w%d" % i

        tiles = [(0, 16)] + [(16 + i * NT, NT) for i in range(cfg.NTILES)]
        for ti, (n0, N) in enumerate(tiles):
            first = (ti == 0)
            p.add("sp", lambda e, n0=n0, N=N: e.dma_start(out=h[:, :, 0:N], in_=xT[:, :, n0:n0 + N]),
                  writes=["h"], dma="h")
            p.add("pool", lambda e: e.tensor_copy(out=ub[:, :, 0:16], in_=carry[:]), reads=["carry"], writes=["ub"])
            emit_rmsnorm(cx, h, "h", KC, 0, N, g1, "g1", ones, oneskey, cfg.eps, ub, "ub", 16,
                         sq, ["sq0", "sq1"], rstd, "rstd")
            p.add("pool", lambda e, N=N: e.tensor_copy(out=carry[:], in_=ub[:, :, N:N + 16]), reads=["ub"], writes=["carry"])
            for gi, w in enumerate(cfg.windows):
                nsteps = int(math.log2(w))
                for cl in range(GC):
                    c = gi * GC + cl
                    src = None
                    lo = 0
                    bufs = [tmpA, tmpB]
                    keys = ["tmpA", "tmpB"]
                    for s_ in range(nsteps):
                        sh = 1 << s_
                        dst = bufs[s_ % 2]
                        dk = keys[s_ % 2]
                        nlo = lo + sh
                        if src is None:
                            p.add("pool", lambda e, dst=dst, c=c, nlo=nlo, sh=sh, N=N: e.tensor_tensor(
                                out=dst[:, nlo:16 + N], in0=ub[:, c, nlo:16 + N], in1=ub[:, c, nlo - sh:16 + N - sh], op=ALU.add),
                                reads=["ub"], writes=[dk])
                        else:
                            sk = keys[(s_ - 1) % 2]
                            p.add("pool", lambda e, dst=dst, src=src, nlo=nlo, sh=sh, N=N: e.tensor_tensor(
                                out=dst[:, nlo:16 + N], in0=src[:, nlo:16 + N], in1=src[:, nlo - sh:16 + N - sh], op=ALU.add),
                                reads=[sk], writes=[dk])
                        src = dst
                        lo = nlo
                    sk = keys[(nsteps - 1) % 2]
                    if first:
                        ok = keys[nsteps % 2]
                        o2 = bufs[nsteps % 2]
                        p.add("pool", lambda e, src=src, o2=o2, gi=gi, N=N: e.tensor_tensor(
                            out=o2[:, 16:16 + N], in0=src[:, 16:16 + N], in1=rc[:, gi, 0:N], op=ALU.mult),
                            reads=[sk, "rc"], writes=[ok])
                        p.add("pool", lambda e, o2=o2, cl=cl, c=c, N=N: e.tensor_tensor(
                            out=xb[:, cl, 0:N], in0=o2[:, 16:16 + N], in1=ub[:, c, 16:16 + N], op=ALU.subtract),
                            reads=[ok, "ub"], writes=["xb"])
                    else:
                        p.add("dve", lambda e, src=src, cl=cl, c=c, w=w, N=N: e.scalar_tensor_tensor(
                            out=xb[:, cl, 0:N], in0=src[:, 16:16 + N], scalar=1.0 / w, in1=ub[:, c, 16:16 + N],
                            op0=ALU.mult, op1=ALU.subtract), reads=[sk, "ub"], writes=["xb"])
                for dt in range(GC):
                    wt, wk, wsem = next_w()
                    dma_w(cx, "pool", wt[:, 0:GC, :], wk, poolw[gi, dt], wsem)
                    b = cx.bank()
                    ps = cx.ps[b]
                    for kc in range(GC):
                        p.add("pe", lambda e, ps=ps, wt=wt, kc=kc, N=N: e.matmul(ps[:, 0:N], wt[:, kc, :], xb[:, kc, 0:N],
                                                                                 start=(kc == 0), stop=(kc == GC - 1)),
                              reads=[wk, "xb"], writes=[("ps", b)])
                    c = gi * GC + dt
                    p.add("dve", lambda e, ps=ps, c=c, N=N: e.scalar_tensor_tensor(
                        out=h[:, c, 0:N], in0=ps[:, 0:N], scalar=psc[:, c:c + 1], in1=h[:, c, 0:N], op0=ALU.mult, op1=ALU.add),
                        reads=[("ps", b), "psc", "h"], writes=["h"])
            emit_rmsnorm(cx, h, "h", KC, 0, N, g2, "g2", ones, oneskey, cfg.eps, ub, "ub", 0,
                         sq, ["sq0", "sq1"], rstd, "rstd")
            emit_ffn(cx, cfg, ub, "ub", 0, N, xb, "xb", h, "h", wg, wu, wd, None, next_w, sg, FT, FG, KC)
            p.add("sp", lambda e, n0=n0, N=N: e.dma_start(out=hT[:, :, n0:n0 + N], in_=h[:, :, 0:N]),
                  reads=["h"], dma="out")
            emit_rmsnorm(cx, h, "h", KC, 0, N, g3, "g3", ones, oneskey, cfg.eps, ub, "ub", 0,
                         sq, ["sq0", "sq1"], rstd, "rstd")
            p.add("sp", lambda e, n0=n0, N=N: e.dma_start(out=uTo[:, :, n0:n0 + N], in_=ub[:, :, 0:N]),
                  reads=["ub"], dma="out")
        p.emit(final_wait_keys=["out"])
    return nc


def emit_ffn(cx, cfg, u, ukey, ucol0, N, xb, xbkey, h, hkey, wg, wu, wd, colscale, next_w, sg, FT, FG, KC):
    p = cx.p
    NG = FT // FG
    for grp in range(NG):
        for fl in range(FG):
            ft = grp * FG + fl
            wgt, wgk, wgs = next_w()
            dma_w(cx, "pool", wgt[:, 0:KC, :], wgk, wg[ft], wgs)
            wut, wuk, wus = next_w()
            dma_w(cx, "pool", wut[:, 0:KC, :], wuk, wu[ft], wus)
            bg = cx.bank()
            bu = cx.bank()
            psg, psu = cx.ps[bg], cx.ps[bu]
            for kc in range(KC):
                p.add("pe", lambda e, psg=psg, wgt=wgt, kc=kc: e.matmul(psg[:, 0:N], wgt[:, kc, :], u[:, kc, ucol0:ucol0 + N],
                                                                        start=(kc == 0), stop=(kc == KC - 1)),
                      reads=[wgk, ukey], writes=[("ps", bg)])
            for kc in range(KC):
                p.add("pe", lambda e, psu=psu, wut=wut, kc=kc: e.matmul(psu[:, 0:N], wut[:, kc, :], u[:, kc, ucol0:ucol0 + N],
                                                                        start=(kc == 0), stop=(kc == KC - 1)),
                      reads=[wuk, ukey], writes=[("ps", bu)])
            s = sg[ft % 2]
            sk = "sg%d" % (ft % 2)
            p.add("act", lambda e, s=s, psg=psg: e.activation(out=s[:, 0:N], in_=psg[:, 0:N], func=AF.Silu),
                  reads=[("ps", bg)], writes=[sk])
            p.add("dve", lambda e, s=s, psu=psu, fl=fl: e.tensor_tensor(out=xb[:, fl, 0:N], in0=psu[:, 0:N], in1=s[:, 0:N],
                                                                        op=ALU.mult),
                  reads=[("ps", bu), sk], writes=[xbkey])
            if colscale is not None:
                cs, csk = colscale
                p.add("dve", lambda e, fl=fl, cs=cs: e.tensor_tensor(out=xb[:, fl, 0:N], in0=xb[:, fl, 0:N], in1=cs,
                                                                      op=ALU.mult),
                      reads=[xbkey, csk], writes=[xbkey])
        for dt in range(KC):
            wt, wk, wsem = next_w()
            dma_w(cx, "pool", wt[:, 0:FG, :], wk, wd[grp, dt], wsem)
            b = cx.bank()
            ps = cx.ps[b]
            for fl in range(FG):
                p.add("pe", lambda e, ps=ps, wt=wt, fl=fl: e.matmul(ps[:, 0:N], wt[:, fl, :], xb[:, fl, 0:N],
                                                                    start=(fl == 0), stop=(fl == FG - 1)),
                      reads=[wk, xbkey], writes=[("ps", b)])
            p.add("dve", lambda e, ps=ps, dt=dt: e.tensor_tensor(out=h[:, dt, 0:N], in0=ps[:, 0:N], in1=h[:, dt, 0:N], op=ALU.add),
                  reads=[("ps", b), hkey], writes=[hkey])


def vec_t(v):
    return np.ascontiguousarray(np.asarray(v, np.float32).reshape(-1, 128).T)


def w_tiles(W):
    K, Fd = W.shape
    return np.ascontiguousarray(np.asarray(W, np.float32).reshape(K // 128, 128, Fd // 128, 128).transpose(2, 1, 0, 3))


def wd_tiles(W, FG):
    Fd, D = W.shape
    NG = Fd // 128 // FG
    return np.ascontiguousarray(np.asarray(W, np.float32).reshape(NG, FG, 128, D // 128, 128).transpose(0, 3, 2, 1, 4))


def to_fm(x):
    T, D = x.shape
    return np.ascontiguousarray(np.asarray(x, np.float32).reshape(T, D // 128, 128).transpose(2, 1, 0))


def from_fm(xT):
    P, KC, T = xT.shape
    return np.ascontiguousarray(xT.transpose(2, 1, 0).reshape(T, KC * 128))


def run_l1(cfg, seqs, meta, norm_mix0, norm_ffn0, pool_w0, pool_scale0, wg0, wu0, wd0, norm_mix1):
    nc = build_l1(cfg)
    GC = cfg.GC
    NW = len(cfg.windows)
    pw = np.asarray(pool_w0, np.float32)
    pwt = np.ascontiguousarray(pw.reshape(NW, GC, 128, GC, 128).transpose(0, 3, 2, 1, 4))
    shared = dict(gmix=vec_t(norm_mix0), gffn=vec_t(norm_ffn0), gnext=vec_t(norm_mix1), pscale=vec_t(pool_scale0), poolw=pwt,
                  wg=w_tiles(wg0), wu=w_tiles(wu0), wd=wd_tiles(wd0, cfg.FG))
    in_maps = [dict(shared, xT=to_fm(s)) for s in seqs]
    res = run_bass_kernel_spmd(nc, in_maps, core_ids=list(range(len(seqs))))
    return [from_fm(r["hT"]) for r in res.results], [r["uTo"] for r in res.results]


class Cfg2:
    def __init__(self, D=4096, HPC=4, LQ=8192, NT=512, eps=1e-6, sub_eps=1e-5, lambda_init=0.0):
        self.D, self.HPC, self.LQ, self.NT = D, HPC, LQ, NT
        self.KC = D // 128
        self.L = 16 + LQ
        self.NB = LQ // 128
        self.NCH = LQ // NT
        self.eps, self.sub_eps, self.lambda_init = eps, sub_eps, lambda_init


def build_l2(cfg):
    nc = bass.Bass("TRN2", target_bir_lowering=False)
    D, HPC, LQ, NT, KC, L, NB, NCH = cfg.D, cfg.HPC, cfg.LQ, cfg.NT, cfg.KC, cfg.L, cfg.NB, cfg.NCH
    BPC = NT // 128
    uT = nc.dram_tensor("uT", [128, KC, L], BF16, kind="ExternalInput").ap()
    wqkv = nc.dram_tensor("wqkv", [HPC, 6, 128, KC, 128], F32, kind="ExternalInput").ap()
    qkn = nc.dram_tensor("qkn", [128, 2], F32, kind="ExternalInput").ap()
    lvec = nc.dram_tensor("lvec", [128, 4], F32, kind="ExternalInput").ap()
    subl = nc.dram_tensor("subl", [128, 2], F32, kind="ExternalInput").ap()
    tri_d = nc.dram_tensor("tri", [128, 128], F32, kind="ExternalInput").ap()
    oT = nc.dram_tensor("oT", [HPC, 2, 128, LQ], F32, kind="ExternalOutput").ap()

    from contextlib import ExitStack
    with ExitStack() as st:
        p = Prog(nc)
        cx = Ctx(nc, st, p)
        ubs = [cx.sb([128, KC, NT], BF16, "ub%d" % i) for i in range(2)]
        sq = [cx.sb([128, NT], F32, "sq%d" % i) for i in range(2)]
        rstd = cx.sb([128, NT], F32, "rstd")
        qk = cx.sb([128, 2], F32, "qk")
        lv = cx.sb([128, 4], F32, "lv")
        sl = cx.sb([128, 2], F32, "sl")
        tri = cx.sb([128, 128], BF16, "tri_sb")
        onesb = cx.sb([128, 128], BF16, "onesb")
        pr = cx.sb([128, 2], F32, "pr")
        ex = cx.sb([128, 2], F32, "ex")
        neglam = cx.sb([128, 1], F32, "neglam")
        consts = emit_consts(cx, [1.0 / 128, 1.0 / 256, 1.0])
        ones128, ones128k = consts[1.0 / 128]
        ones256, ones256k = consts[1.0 / 256]
        ones1, ones1k = consts[1.0]
        p.add("sp", lambda e: e.dma_start(out=qk[:], in_=qkn), writes=["qk"], dma="c2")
        p.add("sp", lambda e: e.dma_start(out=lv[:], in_=lvec), writes=["lv"], dma="c3")
        p.add("sp", lambda e: e.dma_start(out=sl[:], in_=subl), writes=["sl"], dma="c4")
        p.add("pool", lambda e: e.dma_start(out=tri[:], in_=tri_d), writes=["tri"], dma="c5")
        p.add("pool", lambda e: e.memset(onesb[:], 1.0), writes=["onesb"])
        p.add("dve", lambda e: e.tensor_scalar(out=qk[:, 0:1], in0=qk[:, 0:1], scalar1=float(128 ** -0.5), scalar2=None, op0=ALU.mult),
              reads=["qk"], writes=["qk"])
        p.add("dve", lambda e: e.tensor_scalar(out=sl[:], in0=sl[:], scalar1=float(1.0 - cfg.lambda_init), scalar2=None, op0=ALU.mult),
              reads=["sl"], writes=["sl"])
        p.add("dve", lambda e: e.tensor_tensor(out=pr[:, 0:1], in0=lv[:, 0:1], in1=lv[:, 1:2], op=ALU.mult), reads=["lv"], writes=["pr"])
        p.add("dve", lambda e: e.tensor_tensor(out=pr[:, 1:2], in0=lv[:, 2:3], in1=lv[:, 3:4], op=ALU.mult), reads=["lv"], writes=["pr"])
        b = cx.bank()
        psl = cx.ps[b]
        p.add("pe", lambda e: e.matmul(psl[:, 0:2], ones1[:, :], pr[:, 0:2], start=True, stop=True), reads=["pr", ones1k], writes=[("ps", b)])
        p.add("act", lambda e: e.activation(out=ex[:], in_=psl[:, 0:2], func=AF.Exp), reads=[("ps", b)], writes=["ex"])
        p.add("dve", lambda e: e.tensor_tensor(out=neglam[:], in0=ex[:, 1:2], in1=ex[:, 0:1], op=ALU.subtract), reads=["ex"], writes=["neglam"])
        p.add("dve", lambda e: e.tensor_scalar(out=neglam[:], in0=neglam[:], scalar1=float(-cfg.lambda_init), scalar2=None, op0=ALU.add),
              reads=["neglam"], writes=["neglam"])

        tiles = [(0, 16)] + [(16 + i * NT, NT) for i in range(NCH)]
        KT = cx.sb([128, 2, L], BF16, "KT")
        V = cx.sb([128, NB + 1, 256], BF16, "V")
        wsl = [cx.sb([128, KC, 128], BF16, "wq%d" % i) for i in range(2)]
        wv = cx.sb([128, KC, 256], BF16, "wv")
        qc = cx.sb([128, 2, NT], BF16, "qc")
        et = [cx.sb([128, NT], BF16, "et%d" % i) for i in range(3)]
        rden = cx.sb([128, NT], F32, "rden")
        o1n = cx.sb([128, 2, NT], F32, "o1n")
        otmp = cx.sb([128, 2, NT], F32, "otmp")
        ofin, oout = otmp, o1n
        cx.rot = [3, 4, 5, 6, 7]
        cx.ps_i = 0
        ACC = (0, 1, 2)

        def qknorm(ps, psb, N, gcol, out_ap, okey):
            s = sq[0]
            p.add("act", lambda e: e.activation(out=s[:, 0:N], in_=ps[:, 0:N], func=AF.Square), reads=[("ps", psb)], writes=["sq0"])
            b2 = cx.bank()
            ps2 = cx.ps[b2]
            p.add("pe", lambda e: e.matmul(ps2[:, 0:N], ones128[:, :], s[:, 0:N], start=True, stop=True), reads=["sq0", ones128k],
                  writes=[("ps", b2)])
            epst, epskey = get_eps(cx, cfg.eps)
            p.add("act", lambda e: e.activation(out=rstd[:, 0:N], in_=ps2[:, 0:N], func=AF.Sqrt, bias=epst[:, 0:1], scale=1.0),
                  reads=[("ps", b2), epskey], writes=["rstd"])
            p.add("dve", lambda e: e.reciprocal(out=rstd[:, 0:N], in_=rstd[:, 0:N]), reads=["rstd"], writes=["rstd"])
            p.add("dve", lambda e: e.scalar_tensor_tensor(out=out_ap, in0=ps[:, 0:N], scalar=qk[:, gcol:gcol + 1], in1=rstd[:, 0:N],
                                                          op0=ALU.mult, op1=ALU.mult),
                  reads=[("ps", psb), "rstd", "qk"], writes=[okey])

        for hh in range(HPC):
            for i, wi in enumerate((2, 3)):
                dma_w(cx, "pool", wsl[i][:], ("wq", i), wqkv[hh, wi], "wq%d" % i)
            for half in range(2):
                dma_w(cx, "pool", wv[:, :, half * 128:(half + 1) * 128], "wv", wqkv[hh, 4 + half], "wv")
            for ti, (n0, N) in enumerate(tiles):
                ub = ubs[ti % 2]
                ubk = "ub%d" % (ti % 2)
                p.add("sp", lambda e, ub=ub, n0=n0, N=N: e.dma_start(out=ub[:, :, 0:N], in_=uT[:, :, n0:n0 + N]),
                      writes=[ubk], dma="ul%d" % (ti % 2))
                for m in range(2):
                    b = cx.bank()
                    ps = cx.ps[b]
                    for kc in range(KC):
                        p.add("pe", lambda e, ps=ps, ub=ub, m=m, kc=kc, N=N: e.matmul(ps[:, 0:N], wsl[m][:, kc, :], ub[:, kc, 0:N],
                                                                                      start=(kc == 0), stop=(kc == KC - 1)),
                              reads=[("wq", m), ubk], writes=[("ps", b)])
                    qknorm(ps, b, N, 1, KT[:, m, n0:n0 + N], "KT")
                nblk = max(1, N // 128)
                for bi in range(nblk):
                    M = min(128, N)
                    blk = 0 if ti == 0 else 1 + (n0 - 16) // 128 + bi
                    b = cx.bank()
                    ps = cx.ps[b]
                    for kc in range(KC):
                        p.add("pe", lambda e, ps=ps, ub=ub, kc=kc, bi=bi, M=M: e.matmul(
                            ps[0:M, 0:256], ub[:, kc, bi * 128:bi * 128 + M], wv[:, kc, :],
                            start=(kc == 0), stop=(kc == KC - 1)),
                            reads=["wv", ubk], writes=[("ps", b)])
                    p.add("act", lambda e, ps=ps, blk=blk, M=M: e.copy(out=V[0:M, blk, :], in_=ps[0:M, 0:256]), reads=[("ps", b)],
                          writes=["V"])
            for i, wi in enumerate((0, 1)):
                dma_w(cx, "pool", wsl[i][:], ("wq", i), wqkv[hh, wi], "wq%d" % i)
            for c in range(NCH):
                q0 = 16 + c * NT
                ub = ubs[(c + 1) % 2]
                ubk = "ub%d" % ((c + 1) % 2)
                p.add("sp", lambda e, ub=ub, q0=q0: e.dma_start(out=ub[:, :, 0:NT], in_=uT[:, :, q0:q0 + NT]),
                      writes=[ubk], dma="ul%d" % ((c + 1) % 2))
                for m in range(2):
                    b = cx.bank()
                    ps = cx.ps[b]
                    for kc in range(KC):
                        p.add("pe", lambda e, ps=ps, ub=ub, m=m, kc=kc: e.matmul(ps[:, 0:NT], wsl[m][:, kc, :], ub[:, kc, 0:NT],
                                                                                 start=(kc == 0), stop=(kc == KC - 1)),
                              reads=[("wq", m), ubk], writes=[("ps", b)])
                    qknorm(ps, b, NT, 0, qc[:, m, :], "qc")
                for m in range(2):
                    kts = [(0, 16, 0, 0, False)]
                    for kb in range(BPC * c):
                        kts.append((16 + kb * 128, 128, 1 + kb, 0, False))
                    for i in range(BPC):
                        kb = BPC * c + i
                        kts.append((16 + kb * 128, 128, 1 + kb, 128 * i, True))
                    SK = 2
                    nk = len(kts)

                    def emit_score(ki):
                        k0, M, blk, qlo, masked = kts[ki]
                        bs = cx.bank()
                        pss = cx.ps[bs]
                        e_t = et[ki % 3]
                        ek = "et%d" % (ki % 3)
                        p.add("pe", lambda e, pss=pss, m=m, k0=k0, M=M, qlo=qlo: e.matmul(
                            pss[0:M, qlo:NT], KT[:, m, k0:k0 + M], qc[:, m, qlo:NT], start=True, stop=True),
                            reads=["KT", "qc"], writes=[("ps", bs)])
                        p.add("act", lambda e, pss=pss, e_t=e_t, M=M, qlo=qlo: e.activation(out=e_t[0:M, qlo:NT], in_=pss[0:M, qlo:NT],
                                                                                            func=AF.Exp),
                              reads=[("ps", bs)], writes=[ek])
                        if masked:
                            p.add("pool", lambda e, e_t=e_t, qlo=qlo: e.tensor_tensor(out=e_t[:, qlo:qlo + 128], in0=e_t[:, qlo:qlo + 128],
                                                                                      in1=tri[:, :], op=ALU.mult),
                                  reads=[ek, "tri"], writes=[ek])

                    def emit_pv(ki):
                        k0, M, blk, qlo, masked = kts[ki]
                        e_t = et[ki % 3]
                        ek = "et%d" % (ki % 3)
                        first, last = (ki == 0), (ki == nk - 1)
                        for ec in range(2):
                            p.add("pe", lambda e, ec=ec, e_t=e_t, M=M, blk=blk, qlo=qlo, first=first, last=last: e.matmul(
                                cx.ps[ACC[ec]][:, qlo:NT], V[0:M, blk, ec * 128:(ec + 1) * 128], e_t[0:M, qlo:NT], start=first, stop=last),
                                reads=["V", ek], writes=[("ps", ACC[ec])])
                        p.add("pe", lambda e, e_t=e_t, M=M, qlo=qlo, first=first, last=last: e.matmul(
                            cx.ps[ACC[2]][:, qlo:NT], onesb[0:M, :], e_t[0:M, qlo:NT], start=first, stop=last),
                            reads=["onesb", ek], writes=[("ps", ACC[2])])

                    for step in range(nk + SK):
                        if step < nk:
                            emit_score(step)
                        if step >= SK:
                            emit_pv(step - SK)
                    p.add("dve", lambda e: e.reciprocal(out=rden[:, :], in_=cx.ps[ACC[2]][:, 0:NT]), reads=[("ps", ACC[2])], writes=["rden"])
                    dstt, dk = (o1n, "o1n") if m == 0 else (otmp, "otmp")
                    for ec in range(2):
                        p.add("dve", lambda e, ec=ec, dstt=dstt: e.tensor_tensor(out=dstt[:, ec, :], in0=cx.ps[ACC[ec]][:, 0:NT], in1=rden[:, :],
                                                                                 op=ALU.mult),
                              reads=[("ps", ACC[ec]), "rden"], writes=[dk])
                for ec in range(2):
                    p.add("dve", lambda e, ec=ec: e.scalar_tensor_tensor(out=ofin[:, ec, :], in0=otmp[:, ec, :], scalar=neglam[:, 0:1],
                                                                         in1=o1n[:, ec, :], op0=ALU.mult, op1=ALU.add),
                          reads=["otmp", "o1n", "neglam"], writes=["otmp"])
                bsn = cx.bank()
                psn = cx.ps[bsn]
                for ec in range(2):
                    s = sq[ec]
                    p.add("act", lambda e, s=s, ec=ec: e.activation(out=s[:, 0:NT], in_=ofin[:, ec, :], func=AF.Square), reads=["otmp"],
                          writes=["sq%d" % ec])
                    p.add("pe", lambda e, s=s, ec=ec: e.matmul(psn[:, 0:NT], ones256[:, :], s[:, 0:NT], start=(ec == 0), stop=(ec == 1)),
                          reads=["sq%d" % ec, ones256k], writes=[("ps", bsn)])
                epst, epskey = get_eps(cx, cfg.sub_eps)
                p.add("act", lambda e: e.activation(out=rstd[:, 0:NT], in_=psn[:, 0:NT], func=AF.Sqrt, bias=epst[:, 0:1], scale=1.0),
                      reads=[("ps", bsn), epskey], writes=["rstd"])
                p.add("dve", lambda e: e.reciprocal(out=rstd[:, 0:NT], in_=rstd[:, 0:NT]), reads=["rstd"], writes=["rstd"])
                for ec in range(2):
                    p.add("dve", lambda e, ec=ec: e.scalar_tensor_tensor(out=oout[:, ec, :], in0=ofin[:, ec, :], scalar=sl[:, ec:ec + 1],
                                                                         in1=rstd[:, 0:NT], op0=ALU.mult, op1=ALU.mult),
                          reads=["otmp", "rstd", "sl"], writes=["o1n"])
                    p.add("sp", lambda e, ec=ec, hh=hh, c=c: e.dma_start(out=oT[hh, ec, :, c * NT:(c + 1) * NT], in_=oout[:, ec, :]),
                          reads=["o1n"], dma="out")
        p.emit(final_wait_keys=["out"])
    return nc


def qkv_tiles(w_qkv, heads, n_heads):
    W = np.asarray(w_qkv, np.float32)
    D = W.shape[0]
    KC = D // 128
    DQK = n_heads * 256
    out = np.empty((len(heads), 6, 128, KC, 128), np.float32)
    for i, hd in enumerate(heads):
        cols = [hd * 256, hd * 256 + 128, DQK + hd * 256, DQK + hd * 256 + 128, 2 * DQK + hd * 256, 2 * DQK + hd * 256 + 128]
        for j, c0 in enumerate(cols):
            out[i, j] = W[:, c0:c0 + 128].reshape(KC, 128, 128).transpose(1, 0, 2)
    return out


def run_l2(cfg, u_list, w_qkv, q_norm, k_norm, lq1, lk1, lq2, lk2, subln, n_heads):
    nc = build_l2(cfg)
    HPC = cfg.HPC
    ngrp = n_heads // HPC
    tri = np.triu(np.ones((128, 128), np.float32))
    shared = dict(qkn=np.ascontiguousarray(np.stack([q_norm, k_norm], axis=1).astype(np.float32)),
                  lvec=np.ascontiguousarray(np.stack([lq1, lk1, lq2, lk2], axis=1).astype(np.float32)),
                  subl=np.ascontiguousarray(np.asarray(subln, np.float32).reshape(2, 128).T), tri=tri)
    in_maps = []
    wts = [qkv_tiles(w_qkv, list(range(g * HPC, (g + 1) * HPC)), n_heads) for g in range(ngrp)]
    h1_list = u_list
    for b in range(len(u_list)):
        for g in range(ngrp):
            in_maps.append(dict(shared, uT=np.ascontiguousarray(u_list[b]), wqkv=wts[g]))
    res = run_bass_kernel_spmd(nc, in_maps, core_ids=list(range(len(in_maps))))
    outs = []
    i = 0
    for b in range(len(h1_list)):
        parts = []
        for g in range(ngrp):
            o = res.results[i]["oT"]
            i += 1
            parts.append(o.transpose(3, 0, 1, 2).reshape(cfg.LQ, HPC * 256))
        outs.append(np.concatenate(parts, axis=1))
    return outs


class Cfg3:
    def __init__(self, D=4096, FE=3584, NE=8, NT=512, NTILES=4, FG=14, eps=1e-6):
        self.D, self.FE, self.NE, self.NT, self.NTILES, self.FG = D, FE, NE, NT, NTILES, FG
        self.KC = D // 128
        self.FT = FE // 128
        assert self.FT % FG == 0
        self.NG = self.FT // FG
        self.T = NT * NTILES
        self.eps = eps


def build_l3(cfg):
    nc = bass.Bass("TRN2", target_bir_lowering=False)
    D, FE, NE, NT, KC, FT, FG, NG, T = cfg.D, cfg.FE, cfg.NE, cfg.NT, cfg.KC, cfg.FT, cfg.FG, cfg.NG, cfg.T
    hT = nc.dram_tensor("hT", [128, KC, T], F32, kind="ExternalInput").ap()
    oT = nc.dram_tensor("oT", [128, KC, T], F32, kind="ExternalInput").ap()
    gffn = nc.dram_tensor("gffn", [128, KC], F32, kind="ExternalInput").ap()
    wo = nc.dram_tensor("wo", [KC, 128, KC, 128], F32, kind="ExternalInput").ap()
    rt = nc.dram_tensor("rt", [128, KC, NE], F32, kind="ExternalInput").ap()
    ident_d = nc.dram_tensor("ident", [128, 128], F32, kind="ExternalInput").ap()
    weg = nc.dram_tensor("weg", [NE, FT, 128, KC, 128], F32, kind="ExternalInput").ap()
    weu = nc.dram_tensor("weu", [NE, FT, 128, KC, 128], F32, kind="ExternalInput").ap()
    wed = nc.dram_tensor("wed", [NE, NG, KC, 128, FG, 128], F32, kind="ExternalInput").ap()
    outT = nc.dram_tensor("outT", [128, KC, T], F32, kind="ExternalOutput").ap()
    dbg = nc.dram_tensor("dbg", [128, NE, T], F32, kind="ExternalOutput").ap() if getattr(cfg, "debug", False) else None

    from contextlib import ExitStack
    with ExitStack() as st:
        p = Prog(nc)
        cx = Ctx(nc, st, p)
        h = cx.sb([128, KC, NT], F32, "h")
        ub = cx.sb([128, KC, NT], BF16, "ub")
        xb = cx.sb([128, FG, NT], BF16, "xb")
        WS = 6
        wslot = [cx.sb([128, max(KC, FG), 128], BF16, "w%d" % i) for i in range(WS)]
        sq = [cx.sb([128, NT], F32, "sq%d" % i) for i in range(2)]
        rstd = cx.sb([128, NT], F32, "rstd")
        sg = [cx.sb([128, NT], F32, "sg%d" % i) for i in range(2)]
        g2 = cx.sb([128, KC], F32, "g2")
        rtb = cx.sb([128, KC, NE], F32, "rtb")
        uf = [cx.sb([128, 128], F32, "uf%d" % i) for i in range(2)]
        ident = cx.sb([128, 128], F32, "ident_sb")
        combb = cx.sb([128, NE, NT], F32, "combb")
        lg = cx.sb([128, NE], F32, "lg")
        lg2 = cx.sb([128, NE], F32, "lg2")
        eq1 = cx.sb([128, NE], F32, "eq1")
        eq2 = cx.sb([128, NE], F32, "eq2")
        comb_all = cx.sb([128, NT // 128, NE], F32, "comb_all")
        sm = cx.sb([128, 8], F32, "sm")
        diag = [cx.sb([128, 128], F32, "diag%d" % i) for i in range(2)]
        consts = emit_consts(cx, [1.0 / D, 1.0])
        onesD, onesDk = consts[1.0 / D]
        ones1, ones1k = consts[1.0]
        p.add("sp", lambda e: e.dma_start(out=g2[:], in_=gffn), writes=["g2"], dma="c1")
        p.add("sp", lambda e: e.dma_start(out=ident[:], in_=ident_d), writes=["ident"], dma="c2")
        p.add("sp", lambda e: e.dma_start(out=rtb[:], in_=rt), writes=["rtb"], dma="c3")
        wi_ctr = [0]

        def next_w():
            i = wi_ctr[0] % WS
            wi_ctr[0] += 1
            return wslot[i], ("w", i), "w%d" % i

        AX = mybir.AxisListType.X
        for ti in range(cfg.NTILES):
            n0 = ti * NT
            N = NT
            p.add("sp", lambda e, n0=n0: e.dma_start(out=h[:, :, :], in_=hT[:, :, n0:n0 + NT]), writes=["h"], dma="h")
            for half in range(2):
                c0, c1 = half * KC // 2, (half + 1) * KC // 2
                p.add("pool", lambda e, n0=n0, c0=c0, c1=c1: e.dma_start(out=ub[:, c0:c1, :], in_=oT[:, c0:c1, n0:n0 + NT],
                                                                         max_dma_last_dim=8192),
                      writes=["ub"], dma="o")
            for dt in range(KC):
                wt, wk, wsem = next_w()
                dma_w(cx, "pool", wt[:, 0:KC, :], wk, wo[dt], wsem)
                b = cx.bank()
                ps = cx.ps[b]
                for kc in range(KC):
                    p.add("pe", lambda e, ps=ps, wt=wt, kc=kc: e.matmul(ps[:, 0:N], wt[:, kc, :], ub[:, kc, 0:N],
                                                                        start=(kc == 0), stop=(kc == KC - 1)),
                          reads=[wk, "ub"], writes=[("ps", b)])
                p.add("dve", lambda e, ps=ps, dt=dt: e.tensor_tensor(out=h[:, dt, 0:N], in0=ps[:, 0:N], in1=h[:, dt, 0:N], op=ALU.add),
                      reads=[("ps", b), "h"], writes=["h"])
            emit_rmsnorm(cx, h, "h", KC, 0, N, g2, "g2", onesD, onesDk, cfg.eps, ub, "ub", 0, sq, ["sq0", "sq1"], rstd, "rstd")
            cx.rot = [0, 1, 2, 3]
            cb = [4, 5, 6, 7]
            for tb in range(NT // 128):
                b = cx.bank()
                ps = cx.ps[b]
                for kc in range(KC):
                    uft = uf[kc % 2]
                    ufk = "uf%d" % (kc % 2)
                    p.add("dve", lambda e, uft=uft, kc=kc, tb=tb: e.scalar_tensor_tensor(
                        out=uft[:, :], in0=h[:, kc, tb * 128:(tb + 1) * 128], scalar=g2[:, kc:kc + 1],
                        in1=rstd[:, tb * 128:(tb + 1) * 128], op0=ALU.mult, op1=ALU.mult),
                        reads=["h", "g2", "rstd"], writes=[ufk])
                    p.add("pe", lambda e, ps=ps, kc=kc, uft=uft: e.matmul(ps[:, 0:NE], uft[:, :], rtb[:, kc, :],
                                                                          start=(kc == 0), stop=(kc == KC - 1)),
                          reads=[ufk, "rtb"], writes=[("ps", b)])
                p.add("dve", lambda e, ps=ps: e.tensor_copy(out=lg[:], in_=ps[:, 0:NE]), reads=[("ps", b)], writes=["lg"])
                p.add("dve", lambda e: e.tensor_reduce(out=sm[:, 0:1], in_=lg[:], axis=AX, op=ALU.max), reads=["lg"], writes=["sm"])
                p.add("dve", lambda e: e.tensor_scalar(out=eq1[:], in0=lg[:], scalar1=sm[:, 0:1], scalar2=None, op0=ALU.is_equal),
                      reads=["lg", "sm"], writes=["eq1"])
                p.add("dve", lambda e: e.scalar_tensor_tensor(out=lg2[:], in0=eq1[:], scalar=-1e30, in1=lg[:], op0=ALU.mult, op1=ALU.add),
                      reads=["eq1", "lg"], writes=["lg2"])
                p.add("dve", lambda e: e.tensor_reduce(out=sm[:, 1:2], in_=lg2[:], axis=AX, op=ALU.max), reads=["lg2"], writes=["sm"])
                p.add("dve", lambda e: e.tensor_scalar(out=eq2[:], in0=lg2[:], scalar1=sm[:, 1:2], scalar2=None, op0=ALU.is_equal),
                      reads=["lg2", "sm"], writes=["eq2"])
                p.add("dve", lambda e: e.tensor_tensor(out=sm[:, 2:3], in0=sm[:, 1:2], in1=sm[:, 0:1], op=ALU.subtract), reads=["sm"], writes=["sm"])
                p.add("act", lambda e: e.activation(out=sm[:, 3:4], in_=sm[:, 2:3], func=AF.Exp), reads=["sm"], writes=["sm"])
                p.add("dve", lambda e: e.tensor_scalar(out=sm[:, 4:5], in0=sm[:, 3:4], scalar1=1.0, scalar2=None, op0=ALU.add), reads=["sm"], writes=["sm"])
                p.add("dve", lambda e: e.reciprocal(out=sm[:, 5:6], in_=sm[:, 4:5]), reads=["sm"], writes=["sm"])
                p.add("dve", lambda e: e.tensor_tensor(out=sm[:, 6:7], in0=sm[:, 3:4], in1=sm[:, 5:6], op=ALU.mult), reads=["sm"], writes=["sm"])
                p.add("dve", lambda e, tb=tb: e.tensor_scalar(out=comb_all[:, tb, :], in0=eq1[:], scalar1=sm[:, 5:6], scalar2=None, op0=ALU.mult),
                      reads=["eq1", "sm"], writes=["comb"])
                p.add("dve", lambda e, tb=tb: e.scalar_tensor_tensor(out=comb_all[:, tb, :], in0=eq2[:], scalar=sm[:, 6:7], in1=comb_all[:, tb, :], op0=ALU.mult, op1=ALU.add),
                      reads=["eq2", "sm", "comb"], writes=["comb"])
            for eh in range(0, NE, 4):
                for ex_ in range(eh, min(NE, eh + 4)):
                    for tb in range(NT // 128):
                        dg = diag[(ex_ * (NT // 128) + tb) % 2]
                        dk = "diag%d" % ((ex_ * (NT // 128) + tb) % 2)
                        p.add("dve", lambda e, dg=dg, ex_=ex_, tb=tb: e.tensor_scalar(out=dg[:], in0=ident[:], scalar1=comb_all[:, tb, ex_:ex_ + 1],
                                                                                      scalar2=None, op0=ALU.mult),
                              reads=["ident", "comb"], writes=[dk])
                        p.add("pe", lambda e, dg=dg, ex_=ex_, tb=tb: e.matmul(cx.ps[cb[ex_ % 4]][:, tb * 128:(tb + 1) * 128], ones1[:, :], dg[:, :],
                                                                              start=True, stop=True),
                              reads=[dk, ones1k], writes=[("ps", cb[ex_ % 4])])
                    p.add("act", lambda e, ex_=ex_: e.copy(out=combb[:, ex_, :], in_=cx.ps[cb[ex_ % 4]][:, 0:NT]), reads=[("ps", cb[ex_ % 4])],
                          writes=[("combb", ex_)])
            cx.rot = None
            if dbg is not None:
                p.add("sp", lambda e, n0=n0: e.dma_start(out=dbg[:, :, n0:n0 + NT], in_=combb[:, :, :]),
                      reads=[("combb", i) for i in range(NE)], dma="out")
            for ex_ in range(NE):
                emit_ffn(cx, cfg, ub, "ub", 0, N, xb, "xb", h, "h", weg[ex_], weu[ex_], wed[ex_], (combb[:, ex_, :], ("combb", ex_)),
                         next_w, sg, FT, FG, KC)
            p.add("sp", lambda e, n0=n0: e.dma_start(out=outT[:, :, n0:n0 + NT], in_=h[:, :, :]), reads=["h"], dma="out")
        p.emit(final_wait_keys=["out"])
    return nc


def run_l3(cfg, h_list, o_list, norm_ffn1, w_o, router, eg, eu, ed):
    nc = build_l3(cfg)
    NE = cfg.NE
    KC = cfg.KC
    shared = dict(gffn=vec_t(norm_ffn1), wo=w_tiles(w_o),
                  rt=np.ascontiguousarray(np.asarray(router, np.float32).reshape(KC, 128, NE).transpose(1, 0, 2)),
                  ident=np.eye(128, dtype=np.float32),
                  weg=np.stack([w_tiles(eg[e]) for e in range(NE)]),
                  weu=np.stack([w_tiles(eu[e]) for e in range(NE)]),
                  wed=np.stack([wd_tiles(ed[e], cfg.FG) for e in range(NE)]))
    in_maps = [dict(shared, hT=to_fm(hh), oT=to_fm(oo)) for hh, oo in zip(h_list, o_list)]
    res = run_bass_kernel_spmd(nc, in_maps, core_ids=list(range(len(in_maps))))
    if getattr(cfg, "debug", False):
        return [from_fm(r["outT"]) for r in res.results], [r["dbg"] for r in res.results]
    return [from_fm(r["outT"]) for r in res.results]


def kernel(x, meta_tokens, norm_mix, norm_ffn, pool_w, pool_scale, ffn_w_gate, ffn_w_up, ffn_w_down,
           w_qkv, q_norm, k_norm, lambda_q1, lambda_k1, lambda_q2, lambda_k2, subln, w_o,
           router, exp_w_gate, exp_w_up, exp_w_down):
    x = np.asarray(x, np.float32)
    meta = np.asarray(meta_tokens, np.float32)
    B, S, D = x.shape
    NG = N_CORES // B
    per = S // NG
    full = [np.concatenate([meta, x[b]], axis=0) for b in range(B)]
    cfg1 = Cfg1(D=D, F=np.asarray(ffn_w_gate).shape[-1], NT=512, NTILES=per // 512, FG=14)
    seqs = [full[b][per * j: per * j + 16 + per] for b in range(B) for j in range(NG)]
    p1, u1p = run_l1(cfg1, seqs, meta, np.asarray(norm_mix)[0], np.asarray(norm_ffn)[0], np.asarray(pool_w)[0],
                     np.asarray(pool_scale)[0], np.asarray(ffn_w_gate)[0], np.asarray(ffn_w_up)[0], np.asarray(ffn_w_down)[0],
                     np.asarray(norm_mix)[1])
    h1 = [np.concatenate([p1[NG * b][0:16]] + [p1[NG * b + j][16:] for j in range(NG)], axis=0) for b in range(B)]
    u1 = [np.concatenate([u1p[NG * b][:, :, 0:16]] + [u1p[NG * b + j][:, :, 16:] for j in range(NG)], axis=2) for b in range(B)]
    del p1, u1p, seqs, full
    n_heads = 16
    cfg2 = Cfg2(D=D, HPC=n_heads // NG, LQ=S, NT=512, lambda_init=0.8 - 0.6 * math.exp(-0.3 * 1))
    o = run_l2(cfg2, u1, np.asarray(w_qkv)[0], np.asarray(q_norm)[0], np.asarray(k_norm)[0],
               np.asarray(lambda_q1)[0], np.asarray(lambda_k1)[0], np.asarray(lambda_q2)[0], np.asarray(lambda_k2)[0],
               np.asarray(subln)[0], n_heads=n_heads)
    cfg3 = Cfg3(D=D, FE=np.asarray(exp_w_gate).shape[-1], NE=np.asarray(exp_w_gate).shape[1], NT=512, NTILES=per // 512, FG=14)
    h_list = [h1[b][16 + per * j: 16 + per * (j + 1)] for b in range(B) for j in range(NG)]
    o_list = [o[b][per * j: per * (j + 1)] for b in range(B) for j in range(NG)]
    outs = run_l3(cfg3, h_list, o_list, np.asarray(norm_ffn)[1], np.asarray(w_o)[0], np.asarray(router)[0],
                  np.asarray(exp_w_gate)[0], np.asarray(exp_w_up)[0], np.asarray(exp_w_down)[0])
    out = np.stack([np.concatenate(outs[NG * b: NG * (b + 1)], axis=0) for b in range(B)])
    return np.ascontiguousarray(out.astype(np.float32))
```
